# Optimizing a Trainium2 kernel written in Bass

```python
import math
import jax
import jax.numpy as jnp
from jax import lax
import numpy as np

D_MODEL = 1024
BATCH = 8
SEQ = 4096
DEPTH = 4

N_MIXERS = 3
N_LAYERS_A = (DEPTH + 2) // 3
N_LAYERS_B = (DEPTH + 1) // 3
N_LAYERS_C = DEPTH // 3

HEAD_DIM = 64
ROT_DIM = HEAD_DIM // 4
ROPE_THETA = 500000.0
NORM_EPS = 1e-6
NEG_INF = -1e30
Q_BLOCK = 128

DIFF_HEADS = D_MODEL // (2 * HEAD_DIM)
DIFF_QK_WIDTH = DIFF_HEADS * 2 * HEAD_DIM
DIFF_V_DIM = 2 * HEAD_DIM
DIFF_SUBLN_EPS = 1e-5

MOBA_HEADS = D_MODEL // HEAD_DIM
MOBA_BLOCK = 256
MOBA_TOPK = 3

NSA_HEADS = D_MODEL // HEAD_DIM
NSA_GROUPS = NSA_HEADS // 4
NSA_REP = NSA_HEADS // NSA_GROUPS
NSA_Q_BLOCK = 64
CMP_LEN = 32
CMP_STRIDE = 16
CMP_HIDDEN = 4 * HEAD_DIM
SLC_BLOCK = 64
SLC_TOPK = 16
WINDOW = 512
FORCED_SCORE = 1e9
NSA_KV_WIDTH = NSA_GROUPS * HEAD_DIM
NSA_IN_WIDTH = NSA_HEADS * HEAD_DIM + 6 * NSA_KV_WIDTH + 3 * NSA_HEADS

D_FF = 2816

kernel_name = "hybrid_diff_moba_nsa_macaron_trunk"


def rms_norm(x, g, eps=NORM_EPS):
    xf = x.astype(jnp.float32)
    y = xf * lax.rsqrt(jnp.mean(xf * xf, axis=-1, keepdims=True) + eps)
    return (y * g.astype(jnp.float32)).astype(x.dtype)


def partial_rope(x, pos):
    half = ROT_DIM // 2
    inv_freq = ROPE_THETA ** (-jnp.arange(half, dtype=jnp.float32) / half)
    ang = pos.astype(jnp.float32)[:, None] * inv_freq[None, :]
    cos = jnp.cos(ang)[None, :, None, :].astype(x.dtype)
    sin = jnp.sin(ang)[None, :, None, :].astype(x.dtype)
    x1, x2, rest = x[..., :half], x[..., half:ROT_DIM], x[..., ROT_DIM:]
    return jnp.concatenate([x1 * cos - x2 * sin, x2 * cos + x1 * sin, rest], axis=-1)


def masked_probs(scores, mask):
    p = jax.nn.softmax(jnp.where(mask, scores, NEG_INF), axis=-1)
    return jnp.where(mask, p, 0.0)


def gather_blocks(blocks, idx):
    return jax.vmap(jax.vmap(lambda bl, ix: bl[ix]))(blocks, idx)


def chunked(fn, n_chunks):
    o = jnp.moveaxis(lax.map(fn, jnp.arange(n_chunks)), 0, 1)
    return o.reshape((o.shape[0], o.shape[1] * o.shape[2]) + o.shape[3:])


def swiglu(x, w_in, w_out):
    gate, up = jnp.split(x @ w_in, 2, axis=-1)
    return (jax.nn.silu(gate) * up) @ w_out


def diff_attention(h, w_in, w_out, lam, subln_g, lambda_init, pos):
    B, S, _ = h.shape
    H, Dh = DIFF_HEADS, HEAD_DIM
    q, k, v = jnp.split(h @ w_in, 3, axis=-1)
    q = partial_rope(q.reshape(B, S, 2 * H, Dh), pos).reshape(B, S, H, 2, Dh)
    k = partial_rope(k.reshape(B, S, 2 * H, Dh), pos).reshape(B, S, H, 2, Dh)
    v = v.reshape(B, S, H, DIFF_V_DIM)
    lam = lam.astype(jnp.float32)
    lam_full = jnp.exp(jnp.sum(lam[0] * lam[1])) - jnp.exp(jnp.sum(lam[2] * lam[3])) + lambda_init
    scale = Dh ** -0.5

    def chunk(i):
        s0 = i * Q_BLOCK
        qc = lax.dynamic_slice_in_dim(q, s0, Q_BLOCK, axis=1)
        qpos = lax.dynamic_slice_in_dim(pos, s0, Q_BLOCK)
        sc = jnp.einsum('bqhcd,bkhcd->bhcqk', qc, k, preferred_element_type=jnp.float32) * scale
        p = masked_probs(sc, pos[None, :] <= qpos[:, None])
        a = p[:, :, 0] - lam_full * p[:, :, 1]
        return jnp.einsum('bhqk,bkhe->bqhe', a.astype(v.dtype), v)

    o = chunked(chunk, S // Q_BLOCK)
    o = rms_norm(o, subln_g, DIFF_SUBLN_EPS) * (1.0 - lambda_init)
    return o.reshape(B, S, H * DIFF_V_DIM) @ w_out


def moba_attention(h, w_in, w_out, pos):
    B, S, _ = h.shape
    H, Dh, L = MOBA_HEADS, HEAD_DIM, MOBA_BLOCK
    q, k, v = jnp.split(h @ w_in, 3, axis=-1)
    q = partial_rope(q.reshape(B, S, H, Dh), pos)
    k = partial_rope(k.reshape(B, S, H, Dh), pos)
    v = v.reshape(B, S, H, Dh)
    nb = -(-S // L)
    pad = nb * L - S

    def to_blocks(t):
        t = jnp.pad(t, ((0, 0), (0, pad), (0, 0), (0, 0)))
        return t.reshape(B, nb, L, H, Dh).transpose(0, 3, 1, 2, 4)

    kb, vb = to_blocks(k), to_blocks(v)
    k_mean = jnp.mean(kb.astype(jnp.float32), axis=3)
    topk = min(MOBA_TOPK, nb)
    blk_ids = jnp.arange(nb)
    scale = Dh ** -0.5

    def chunk(i):
        s0 = i * Q_BLOCK
        qidx = s0 + jnp.arange(Q_BLOCK)
        cur = s0 // L
        qc = lax.dynamic_slice_in_dim(q, s0, Q_BLOCK, axis=1)
        gate = jnp.einsum('bqhd,bhnd->bhqn', qc.astype(jnp.float32), k_mean)
        gate = jnp.where(blk_ids < cur, gate, NEG_INF)
        top_s, top_i = lax.top_k(gate, topk)
        sel_mask = jnp.broadcast_to((top_s > 0.5 * NEG_INF)[..., None],
                                    top_s.shape + (L,)).reshape(B, H, Q_BLOCK, topk * L)
        ksel = gather_blocks(kb, top_i).reshape(B, H, Q_BLOCK, topk * L, Dh)
        vsel = gather_blocks(vb, top_i).reshape(B, H, Q_BLOCK, topk * L, Dh)
        kown = lax.dynamic_index_in_dim(kb, cur, axis=2, keepdims=False)
        vown = lax.dynamic_index_in_dim(vb, cur, axis=2, keepdims=False)
        own_mask = jnp.broadcast_to((cur * L + jnp.arange(L))[None, :] <= qidx[:, None],
                                    (B, H, Q_BLOCK, L))
        s_sel = jnp.einsum('bqhd,bhqkd->bhqk', qc, ksel, preferred_element_type=jnp.float32)
        s_own = jnp.einsum('bqhd,bhkd->bhqk', qc, kown, preferred_element_type=jnp.float32)
        scores = jnp.concatenate([s_sel, s_own], axis=-1) * scale
        p = masked_probs(scores, jnp.concatenate([sel_mask, own_mask], axis=-1)).astype(v.dtype)
        return (jnp.einsum('bhqk,bhqkd->bqhd', p[..., :topk * L], vsel)
                + jnp.einsum('bhqk,bhkd->bqhd', p[..., topk * L:], vown))

    o = chunked(chunk, S // Q_BLOCK)
    return o.reshape(B, S, H * Dh) @ w_out


def compress(t, pe, w1, w2):
    B, S, G, Dh = t.shape
    r = CMP_LEN // CMP_STRIDE
    c = t.reshape(B, S // CMP_STRIDE, CMP_STRIDE, G, Dh)
    nc = S // CMP_STRIDE - r + 1
    blocks = jnp.concatenate([c[:, j:j + nc] for j in range(r)], axis=2)
    blocks = blocks + pe[None, None, :, None, :].astype(t.dtype)
    flat = blocks.transpose(0, 1, 3, 2, 4).reshape(B, nc, G, CMP_LEN * Dh)
    return jax.nn.silu(flat @ w1) @ w2


def nsa_attention(h, w_in, w_out, cmp_pe, cmp_w1, cmp_w2, pos):
    B, S, _ = h.shape
    H, G, R, Dh = NSA_HEADS, NSA_GROUPS, NSA_REP, HEAD_DIM
    Qb = NSA_Q_BLOCK
    splits = [H * Dh + j * NSA_KV_WIDTH for j in range(7)]
    q, kc, vc, ks, vs, kw, vw, g_logits = jnp.split(h @ w_in, splits, axis=-1)
    q = partial_rope(q.reshape(B, S, H, Dh), pos).reshape(B, S, G, R, Dh)

    def kv(t):
        return t.reshape(B, S, G, Dh)

    kcmp = compress(partial_rope(kv(kc), pos), cmp_pe[0], cmp_w1[0], cmp_w2[0])
    vcmp = compress(kv(vc), cmp_pe[1], cmp_w1[1], cmp_w2[1])
    nc = kcmp.shape[1]
    cmp_end = jnp.arange(nc) * CMP_STRIDE + CMP_LEN - 1
    nsel = S // SLC_BLOCK
    n_top = min(SLC_TOPK, nsel)
    cmp_start = jnp.arange(nc)[:, None] * CMP_STRIDE
    blk_start = jnp.arange(nsel)[None, :] * SLC_BLOCK
    overlap = ((cmp_start < blk_start + SLC_BLOCK) & (cmp_start + CMP_LEN > blk_start)).astype(jnp.float32)

    def sel_blocks(t):
        return t.reshape(B, nsel, SLC_BLOCK, G, Dh).transpose(0, 3, 1, 2, 4)

    kb = sel_blocks(partial_rope(kv(ks), pos))
    vb = sel_blocks(kv(vs))
    def padw(t):
        return jnp.pad(t, ((0, 0), (WINDOW, 0), (0, 0), (0, 0)))

    kwp = padw(partial_rope(kv(kw), pos))
    vwp = padw(kv(vw))
    gates = jax.nn.sigmoid(g_logits).reshape(B, S, G, R, 3)
    blk_ids = jnp.arange(nsel)
    scale = Dh ** -0.5

    def chunk(i):
        s0 = i * Qb
        qidx = s0 + jnp.arange(Qb)
        qc = lax.dynamic_slice_in_dim(q, s0, Qb, axis=1)
        s_c = jnp.einsum('bqgrd,bngd->bgrqn', qc, kcmp, preferred_element_type=jnp.float32) * scale
        p_c = masked_probs(s_c, cmp_end[None, :] <= qidx[:, None])
        o_c = jnp.einsum('bgrqn,bngd->bqgrd', p_c.astype(vcmp.dtype), vcmp)
        imp = jnp.einsum('bgrqn,nj->bgqj', p_c, overlap)
        qblk = (qidx // SLC_BLOCK)[:, None]
        forced = (blk_ids == 0) | (blk_ids == qblk) | (blk_ids == qblk - 1)
        imp = jnp.where(blk_ids <= qblk, jnp.where(forced, FORCED_SCORE, imp), NEG_INF)
        top_s, top_i = lax.top_k(imp, n_top)
        kidx = top_i[..., None] * SLC_BLOCK + jnp.arange(SLC_BLOCK)
        m_s = ((top_s > 0.5 * NEG_INF)[..., None] & (kidx <= qidx[:, None, None])
               ).reshape(B, G, 1, Qb, n_top * SLC_BLOCK)
        ksel = gather_blocks(kb, top_i).reshape(B, G, Qb, n_top * SLC_BLOCK, Dh)
        vsel = gather_blocks(vb, top_i).reshape(B, G, Qb, n_top * SLC_BLOCK, Dh)
        s_s = jnp.einsum('bqgrd,bgqkd->bgrqk', qc, ksel, preferred_element_type=jnp.float32) * scale
        p_s = masked_probs(s_s, m_s)
        o_s = jnp.einsum('bgrqk,bgqkd->bqgrd', p_s.astype(vsel.dtype), vsel)
        kwc = lax.dynamic_slice_in_dim(kwp, s0, WINDOW + Qb, axis=1)
        vwc = lax.dynamic_slice_in_dim(vwp, s0, WINDOW + Qb, axis=1)
        kidx_w = s0 - WINDOW + jnp.arange(WINDOW + Qb)
        dist = qidx[:, None] - kidx_w[None, :]
        m_w = (kidx_w[None, :] >= 0) & (dist >= 0) & (dist < WINDOW)
        s_w = jnp.einsum('bqgrd,bkgd->bgrqk', qc, kwc, preferred_element_type=jnp.float32) * scale
        p_w = masked_probs(s_w, m_w)
        o_w = jnp.einsum('bgrqk,bkgd->bqgrd', p_w.astype(vwc.dtype), vwc)
        gc = lax.dynamic_slice_in_dim(gates, s0, Qb, axis=1)
        return gc[..., 0:1] * o_c + gc[..., 1:2] * o_s + gc[..., 2:3] * o_w

    o = chunked(chunk, S // Qb)
    return o.reshape(B, S, H * Dh) @ w_out


def setup_inputs(seed: int = 0) -> dict:
    key = jax.random.key(seed)
    ks = jax.random.split(key, 15)

    def nrm(k, shape):
        return jax.random.normal(k, shape, jnp.float32)

    def w(k, shape, fan_in):
        return nrm(k, shape) * fan_in ** -0.5

    return {
        "x": nrm(ks[0], (BATCH, SEQ, D_MODEL)),
        "norm_g": 1.0 + 0.05 * nrm(ks[1], (DEPTH, 6, D_MODEL)),
        "ffn_w_in": w(ks[2], (DEPTH, 2, D_MODEL, 2 * D_FF), D_MODEL),
        "ffn_w_out": w(ks[3], (DEPTH, 2, D_FF, D_MODEL), D_FF),
        "diff_w_in": w(ks[4], (N_LAYERS_A, D_MODEL, 3 * DIFF_QK_WIDTH), D_MODEL),
        "diff_w_out": w(ks[5], (N_LAYERS_A, DIFF_HEADS * DIFF_V_DIM, D_MODEL), DIFF_HEADS * DIFF_V_DIM),
        "diff_lambda": 0.1 * nrm(ks[6], (N_LAYERS_A, 4, HEAD_DIM)),
        "diff_subln": 1.0 + 0.05 * nrm(ks[7], (N_LAYERS_A, DIFF_V_DIM)),
        "moba_w_in": w(ks[8], (N_LAYERS_B, D_MODEL, 3 * MOBA_HEADS * HEAD_DIM), D_MODEL),
        "moba_w_out": w(ks[9], (N_LAYERS_B, MOBA_HEADS * HEAD_DIM, D_MODEL), MOBA_HEADS * HEAD_DIM),
        "nsa_w_in": w(ks[10], (N_LAYERS_C, D_MODEL, NSA_IN_WIDTH), D_MODEL),
        "nsa_w_out": w(ks[11], (N_LAYERS_C, NSA_HEADS * HEAD_DIM, D_MODEL), NSA_HEADS * HEAD_DIM),
        "nsa_cmp_pe": 0.2 * nrm(ks[12], (N_LAYERS_C, 2, CMP_LEN, HEAD_DIM)),
        "nsa_cmp_w1": w(ks[13], (N_LAYERS_C, 2, CMP_LEN * HEAD_DIM, CMP_HIDDEN), CMP_LEN * HEAD_DIM),
        "nsa_cmp_w2": w(ks[14], (N_LAYERS_C, 2, CMP_HIDDEN, HEAD_DIM), CMP_HIDDEN),
    }


def reference(x, norm_g, ffn_w_in, ffn_w_out, diff_w_in, diff_w_out, diff_lambda, diff_subln,
              moba_w_in, moba_w_out, nsa_w_in, nsa_w_out, nsa_cmp_pe, nsa_cmp_w1, nsa_cmp_w2):
    pos = jnp.arange(x.shape[1])
    h = x
    for i in range(DEPTH):
        g = norm_g[i]
        h = h + 0.5 * rms_norm(swiglu(rms_norm(h, g[0]), ffn_w_in[i, 0], ffn_w_out[i, 0]), g[1])
        m = rms_norm(h, g[2])
        kind, j = i % N_MIXERS, i // N_MIXERS
        if kind == 0:
            lambda_init = 0.8 - 0.6 * math.exp(-0.3 * i)
            m = diff_attention(m, diff_w_in[j], diff_w_out[j], diff_lambda[j], diff_subln[j],
                               lambda_init, pos)
        elif kind == 1:
            m = moba_attention(m, moba_w_in[j], moba_w_out[j], pos)
        else:
            m = nsa_attention(m, nsa_w_in[j], nsa_w_out[j], nsa_cmp_pe[j], nsa_cmp_w1[j],
                              nsa_cmp_w2[j], pos)
        h = h + rms_norm(m, g[3])
        h = h + 0.5 * rms_norm(swiglu(rms_norm(h, g[4]), ffn_w_in[i, 1], ffn_w_out[i, 1]), g[5])
    return h
```

```python
from contextlib import ExitStack
import math
import numpy as np
import ml_dtypes
import concourse.bass as bass
import concourse.mybir as mybir
from concourse.bass_utils import run_bass_kernel_spmd

F32 = mybir.dt.float32
BF16 = mybir.dt.bfloat16
AF = mybir.ActivationFunctionType
ALU = mybir.AluOpType
AX = mybir.AxisListType

S = 4096
D = 1024
DFF = 2816
DEPTH = 4
NT = S // 128
HD = 64
NEG = -30000.0
EPS = 1e-6


class Buf:
    __slots__ = ("w", "r")

    def __init__(self):
        self.w = None
        self.r = []


class SemCtr:
    __slots__ = ("sem", "val", "idx")

    def __init__(self, sem, idx):
        self.sem = sem
        self.val = 0
        self.idx = idx


class Eng:
    def __init__(self, fw, name, eng, is_compute=True, self_sync=True):
        self.fw = fw
        self.name = name
        self.eng = eng
        self.seen = {}
        self.self_sync = self_sync
        self.ctr = fw.new_sem("s_" + name) if is_compute else None
        self.dma_sems = []
        self.dma_rr = 0

    def _wait(self, tok, raw=True):
        s, v = tok
        if s is self.ctr and not (self.self_sync and raw):
            return
        if self.seen.get(s.idx, 0) >= v:
            return
        self.eng.wait_ge(s.sem, v)
        self.seen[s.idx] = v
        self.fw.n_wait += 1

    def deps(self, reads, writes):
        for b in reads:
            if b.w is not None:
                self._wait(b.w, True)
        for b in writes:
            if b.w is not None:
                self._wait(b.w, False)
            for t in b.r:
                self._wait(t, False)

    def done(self, tok, reads, writes):
        for b in writes:
            b.w = tok
            b.r = []
        for b in reads:
            if b in writes:
                continue
            b.r = [t for t in b.r if t[0] is not tok[0]] + [tok]

    def op(self, fn, reads=(), writes=()):
        self.deps(reads, writes)
        inst = fn()
        self.ctr.val += 1
        inst.then_inc(self.ctr.sem, 1)
        tok = (self.ctr, self.ctr.val)
        self.done(tok, reads, writes)
        self.fw.n_inst += 1
        return tok

    def dma(self, out, in_, reads=(), writes=(), **kw):
        if not self.dma_sems:
            self.dma_sems = [self.fw.new_sem(f"d_{self.name}{i}") for i in range(self.fw.n_dma_sems)]
        s = self.dma_sems[self.dma_rr % len(self.dma_sems)]
        self.dma_rr += 1
        if s.val > 0:
            self._wait((s, s.val))
        self.deps(reads, writes)
        inst = self.eng.dma_start(out=out, in_=in_, **kw)
        s.val += 16
        inst.then_inc(s.sem, 16)
        tok = (s, s.val)
        self.done(tok, reads, writes)
        self.fw.n_inst += 1
        return tok


class Tile:
    def __init__(self, t):
        self.t = t
        self.b = Buf()

    def __getitem__(self, idx):
        return self.t[idx]


class FW:
    def __init__(self, nc, n_dma_sems=8):
        self.nc = nc
        self.stack = [ExitStack()]
        self.n_dma_sems = n_dma_sems
        self.n_sem = 0
        self.n_inst = 0
        self.n_wait = 0
        self.sems = []
        self.pe = Eng(self, "pe", nc.tensor, self_sync=False)
        self.act = Eng(self, "act", nc.scalar)
        self.dve = Eng(self, "dve", nc.vector)
        self.pool = Eng(self, "pool", nc.gpsimd)
        self.sp = Eng(self, "sp", nc.sync, is_compute=False)
        self.engs = [self.pe, self.act, self.dve, self.pool, self.sp]
        self._uid = 0

    def new_sem(self, name):
        h = self.stack[0].enter_context(self.nc.semaphore(name))
        self.n_sem += 1
        sc = SemCtr(h, self.n_sem)
        self.sems.append(sc)
        return sc

    def push(self):
        self.stack.append(ExitStack())

    def pop(self):
        self.barrier()
        self.stack.pop().close()
        for e in (self.pe, self.act):
            e.ctr = self.new_sem(f"s_{e.name}_{self.n_sem}")

    def barrier(self):
        for e in self.engs:
            for s in self.sems:
                if s.val > 0:
                    e._wait((s, s.val), True)

    def sb(self, shape, dtype):
        self._uid += 1
        return Tile(self.stack[-1].enter_context(self.nc.sbuf_tensor(f"sb{self._uid}", list(shape), dtype)))

    def ps(self, shape, dtype):
        self._uid += 1
        return Tile(self.stack[-1].enter_context(self.nc.psum_tensor(f"ps{self._uid}", list(shape), dtype)))

    def close(self):
        self.barrier()
        while self.stack:
            self.stack.pop().close()


class Prog:
    def __init__(self, nc, fw, io):
        self.nc = nc
        self.fw = fw
        self.io = io
        nc_ = nc
        fw_ = fw
        self.ident = fw.sb([128, 128], BF16)
        self.identf = fw.sb([128, 128], F32)
        self.mhalf = fw.sb([128, 1], F32)
        self.ones_bf = fw.sb([128, 128], BF16)
        self.junk = fw.sb([128, D], BF16)
        fw.pool.op(lambda: nc_.gpsimd.memset(self.identf[:], 1.0), writes=[self.identf.b])
        fw.pool.op(lambda: nc_.gpsimd.affine_select(out=self.identf[:], in_=self.identf[:], pattern=[[-1, 128]],
                                                     compare_op=ALU.is_equal, fill=0.0, base=0, channel_multiplier=1),
                   reads=[self.identf.b], writes=[self.identf.b])
        fw.dve.op(lambda: nc_.vector.tensor_copy(out=self.ident[:], in_=self.identf[:]),
                  reads=[self.identf.b], writes=[self.ident.b])
        fw.dve.op(lambda: nc_.vector.memset(self.mhalf[:], -0.5), writes=[self.mhalf.b])
        fw.dve.op(lambda: nc_.vector.memset(self.ones_bf[:], 1.0), writes=[self.ones_bf.b])

    def rstd(self, ss, out, mul, eng_pool=True):
        raise NotImplementedError

    def load_gT(self, dst, vec_ap):
        self.fw.sp.dma(dst[:], vec_ap.rearrange("(k p) -> p k", p=128), writes=[dst.b],
                       allow_slow_non_contiguous=True)

    def load_bc(self, dst, row_ap):
        self.fw.sp.dma(dst[:], row_ap.partition_broadcast(128), writes=[dst.b])

    def norm_to_xnT(self, tt, src, g_T, htile, ss, var, rs, xn, ptr, xnT, dmaq=None):
        nc, fw = self.nc, self.fw
        fw.sp.dma(htile[:], src[tt * 128:(tt + 1) * 128, :], writes=[htile.b])
        fw.act.op(lambda: nc.scalar.activation(out=self.junk[:], in_=htile[:], func=AF.Square, accum_out=ss[:]),
                  reads=[htile.b], writes=[self.junk.b, ss.b])
        fw.dve.op(lambda: nc.vector.tensor_scalar(out=var[:], in0=ss[:], scalar1=1.0 / D, scalar2=EPS,
                                                  op0=ALU.mult, op1=ALU.add), reads=[ss.b], writes=[var.b])
        fw.pool.op(lambda: nc.gpsimd.tensor_tensor(out=rs[:], in0=var[:], in1=self.mhalf[:], op=ALU.pow),
                   reads=[var.b, self.mhalf.b], writes=[rs.b])
        fw.pool.op(lambda: nc.gpsimd.tensor_scalar(out=xn[:], in0=htile[:], scalar1=rs[:], scalar2=None,
                                                   op0=ALU.mult), reads=[htile.b, rs.b], writes=[xn.b])
        for k in range(8):
            fw.pe.op(lambda k=k: nc.tensor.transpose(out=ptr[:, k, :], in_=xn[:, k * 128:(k + 1) * 128],
                                                     identity=self.ident[:]),
                     reads=[xn.b, self.ident.b], writes=[ptr.b])
        fw.dve.op(lambda: nc.vector.tensor_tensor(out=xnT[:], in0=ptr[:, 0:8, :],
                                                  in1=g_T[:, :, None].to_broadcast([128, 8, 128]), op=ALU.mult),
                  reads=[ptr.b, g_T.b], writes=[xnT.b])

    def ffn(self, src, dst, w_in, w_out, g_pre, g_post):
        nc, fw = self.nc, self.fw
        fw.push()
        win = [fw.sb([128, 2 * DFF], BF16) for _ in range(8)]
        wout = [fw.sb([128, D], BF16) for _ in range(22)]
        for k in range(8):
            fw.pool.dma(win[k][:], w_in[k * 128:(k + 1) * 128, :], writes=[win[k].b])
        for c in range(22):
            fw.pool.dma(wout[c][:], w_out[c * 128:(c + 1) * 128, :], writes=[wout[c].b])
        gT = fw.sb([128, 8], F32)
        self.load_gT(gT, g_pre)
        gbc = fw.sb([128, D], F32)
        self.load_bc(gbc, g_post)
        NH = 3
        htiles = [fw.sb([128, D], F32) for _ in range(NH)]
        xn = fw.sb([128, D], BF16)
        xnT = fw.sb([128, 8, 128], BF16)
        sg = [fw.sb([128, 512], BF16) for _ in range(2)]
        act = fw.sb([128, DFF], BF16)
        actT = fw.sb([128, 22, 128], BF16)
        tt_ = [fw.sb([128, D], F32) for _ in range(2)]
        ss = [fw.sb([128, 1], F32) for _ in range(2)]
        var = [fw.sb([128, 1], F32) for _ in range(2)]
        rs = [fw.sb([128, 1], F32) for _ in range(2)]
        ss2 = [fw.sb([128, 1], F32) for _ in range(2)]
        var2 = [fw.sb([128, 1], F32) for _ in range(2)]
        rs2 = [fw.sb([128, 1], F32) for _ in range(2)]
        junk = self.junk
        pg = [fw.ps([128, 512], F32) for _ in range(2)]
        pu = [fw.ps([128, 512], F32) for _ in range(2)]
        py = fw.ps([128, D], F32)
        ptr = [fw.ps([128, 8, 128], BF16) for _ in range(2)]
        groups = [(c0, 512) for c0 in range(0, 2560, 512)] + [(2560, 256)]
        state = {"gi": 0, "tr": 0}

        def stage_norm(i):
            p = ptr[state["tr"] % 2]
            state["tr"] += 1
            self.norm_to_xnT(i, src, gT, htiles[i % NH], ss[i % 2], var[i % 2], rs[i % 2], xn, p, xnT)

        def stage_m1(i):
            for (c0, w) in groups:
                gi = state["gi"]
                state["gi"] += 1
                a, b_ = pg[gi % 2], pu[gi % 2]
                for k in range(8):
                    fw.pe.op(lambda k=k: nc.tensor.matmul(a[:, 0:w], lhsT=xnT[:, k, :], rhs=win[k][:, c0:c0 + w],
                                                          start=(k == 0), stop=(k == 7)),
                             reads=[xnT.b, win[k].b], writes=[a.b])
                for k in range(8):
                    fw.pe.op(lambda k=k: nc.tensor.matmul(b_[:, 0:w], lhsT=xnT[:, k, :],
                                                          rhs=win[k][:, DFF + c0:DFF + c0 + w],
                                                          start=(k == 0), stop=(k == 7)),
                             reads=[xnT.b, win[k].b], writes=[b_.b])
                s_ = sg[gi % 2]
                fw.act.op(lambda: nc.scalar.activation(out=s_[:, 0:w], in_=a[:, 0:w], func=AF.Silu),
                          reads=[a.b], writes=[s_.b])
                fw.dve.op(lambda: nc.vector.tensor_tensor(out=act[:, c0:c0 + w], in0=s_[:, 0:w], in1=b_[:, 0:w],
                                                          op=ALU.mult), reads=[s_.b, b_.b], writes=[act.b])

        def stage_t22(i):
            for r0 in (0, 8, 16):
                n = min(8, 22 - r0)
                p = ptr[state["tr"] % 2]
                state["tr"] += 1
                for c in range(n):
                    fw.pe.op(lambda c=c: nc.tensor.transpose(out=p[:, c, :], in_=act[:, (r0 + c) * 128:(r0 + c + 1) * 128],
                                                             identity=self.ident[:]),
                             reads=[act.b, self.ident.b], writes=[p.b])
                if r0 == 8:
                    fw.act.op(lambda: nc.scalar.copy(out=actT[:, r0:r0 + n, :], in_=p[:, 0:n, :]),
                              reads=[p.b], writes=[actT.b])
                else:
                    fw.dve.op(lambda: nc.vector.tensor_copy(out=actT[:, r0:r0 + n, :], in_=p[:, 0:n, :]),
                              reads=[p.b], writes=[actT.b])

        def stage_m2(i):
            for hh in range(2):
                for c in range(22):
                    fw.pe.op(lambda c=c: nc.tensor.matmul(py[:, hh * 512:(hh + 1) * 512], lhsT=actT[:, c, :],
                                                          rhs=wout[c][:, hh * 512:(hh + 1) * 512],
                                                          start=(c == 0), stop=(c == 21)),
                             reads=[actT.b, wout[c].b], writes=[py.b])
            j = i % 2
            ht = htiles[i % NH]
            fw.act.op(lambda: nc.scalar.activation(out=junk[:], in_=py[:], func=AF.Square, accum_out=ss2[j][:]),
                      reads=[py.b], writes=[junk.b, ss2[j].b])
            fw.dve.op(lambda: nc.vector.tensor_scalar(out=var2[j][:], in0=ss2[j][:], scalar1=1.0 / D, scalar2=EPS,
                                                      op0=ALU.mult, op1=ALU.add), reads=[ss2[j].b], writes=[var2[j].b])
            fw.pool.op(lambda: nc.gpsimd.tensor_tensor(out=rs2[j][:], in0=var2[j][:], in1=self.mhalf[:], op=ALU.pow),
                       reads=[var2[j].b, self.mhalf.b], writes=[rs2[j].b])
            t = tt_[j]
            fw.dve.op(lambda: nc.vector.tensor_tensor(out=t[:], in0=py[:], in1=gbc[:], op=ALU.mult),
                      reads=[py.b, gbc.b], writes=[t.b])
            fw.pool.op(lambda: nc.gpsimd.tensor_scalar(out=t[:], in0=t[:], scalar1=rs2[j][:], scalar2=0.5,
                                                       op0=ALU.mult, op1=ALU.mult),
                       reads=[t.b, rs2[j].b], writes=[t.b])
            fw.pool.op(lambda: nc.gpsimd.tensor_tensor(out=t[:], in0=t[:], in1=ht[:], op=ALU.add),
                       reads=[t.b, ht.b], writes=[t.b])
            fw.sp.dma(dst[i * 128:(i + 1) * 128, :], t[:], reads=[t.b])

        stage_norm(0)
        for i in range(NT):
            stage_m1(i)
            if i + 1 < NT:
                stage_norm(i + 1)
            if i >= 1:
                stage_m2(i - 1)
            stage_t22(i)
        stage_m2(NT - 1)
        fw.pop()


    def proj(self, src, w_in, W, g_pre, segs, fm_dst, tm_dst, cs_tab):
        nc, fw = self.nc, self.fw
        fw.push()
        wt = [fw.sb([128, W], BF16) for _ in range(8)]
        for k in range(8):
            fw.pool.dma(wt[k][:], w_in[k * 128:(k + 1) * 128, :], writes=[wt[k].b])
        gT = fw.sb([128, 8], F32)
        self.load_gT(gT, g_pre)
        cc = fw.sb([128, NT, 16], F32)
        sn = fw.sb([128, NT, 16], F32)
        fw.sp.dma(cc[:], cs_tab[0].rearrange("(t p) c -> p t c", p=128), writes=[cc.b])
        fw.sp.dma(sn[:], cs_tab[1].rearrange("(t p) c -> p t c", p=128), writes=[sn.b])
        nseg = len(segs)
        nfm = sum(1 for kd, _ in segs if kd in ("rope", "fm", "gate")) * 2
        ntm = sum(1 for kd, _ in segs if kd == "tm") * 256
        htiles = [fw.sb([128, D], F32) for _ in range(2)]
        xn = fw.sb([128, D], BF16)
        xnT = [fw.sb([128, 8, 128], BF16) for _ in range(2)]
        ss = [fw.sb([128, 1], F32) for _ in range(2)]
        var = [fw.sb([128, 1], F32) for _ in range(2)]
        rs = [fw.sb([128, 1], F32) for _ in range(2)]
        tm = [fw.sb([128, nseg * 256], BF16) for _ in range(2)]
        ra = [fw.sb([128, 4, 16], F32) for _ in range(2)]
        rb = [fw.sb([128, 4, 16], F32) for _ in range(2)]
        stT = [fw.sb([128, nfm, 512], BF16) for _ in range(2)]
        ptr = fw.ps([128, 8, 128], BF16)
        ptr2 = [fw.ps([128, 8, 128], BF16) for _ in range(2)]
        pp = [fw.ps([128, 512], F32) for _ in range(3)]
        ngrp = (nseg + 1) // 2
        cnt = {"g": 0, "t": 0, "r": 0}
        fm_view = fm_dst.rearrange("(c p) t -> p c t", p=128)
        for i in range(NT):
            xT = xnT[i % 2]
            self.norm_to_xnT(i, src, gT, htiles[i % 2], ss[i % 2], var[i % 2], rs[i % 2], xn, ptr, xT)
            tmi = tm[i % 2]
            for g in range(ngrp):
                c0 = g * 512
                w = min(512, W - c0)
                p_ = pp[cnt["g"] % 3]
                cnt["g"] += 1
                for k in range(8):
                    fw.pe.op(lambda k=k: nc.tensor.matmul(p_[:, 0:w], lhsT=xT[:, k, :], rhs=wt[k][:, c0:c0 + w],
                                                          start=(k == 0), stop=(k == 7)),
                             reads=[xT.b, wt[k].b], writes=[p_.b])
                for sidx in (2 * g, 2 * g + 1):
                    if sidx >= nseg:
                        continue
                    kind = segs[sidx][0]
                    lo = (sidx % 2) * 256
                    wseg = min(256, w - lo)
                    dstv = tmi[:, sidx * 256: sidx * 256 + wseg]
                    if kind == "rope":
                        x3 = p_[:, lo:lo + 256].rearrange("p (h d) -> p h d", d=64)
                        o3 = tmi[:, sidx * 256:(sidx + 1) * 256].rearrange("p (h d) -> p h d", d=64)
                        a_, b_ = ra[cnt["r"] % 2], rb[cnt["r"] % 2]
                        cnt["r"] += 1
                        ccb = cc[:, i, :][:, None, :].to_broadcast([128, 4, 16])
                        fw.dve.op(lambda: nc.vector.tensor_tensor(out=a_[:], in0=x3[:, :, 0:16], in1=ccb, op=ALU.mult),
                                  reads=[p_.b, cc.b], writes=[a_.b])
                        fw.dve.op(lambda: nc.vector.tensor_tensor(out=b_[:, :, 0:8], in0=x3[:, :, 8:16],
                                                                  in1=sn[:, i, 0:8][:, None, :].to_broadcast([128, 4, 8]),
                                                                  op=ALU.mult), reads=[p_.b, sn.b], writes=[b_.b])
                        fw.dve.op(lambda: nc.vector.tensor_tensor(out=b_[:, :, 8:16], in0=x3[:, :, 0:8],
                                                                  in1=sn[:, i, 8:16][:, None, :].to_broadcast([128, 4, 8]),
                                                                  op=ALU.mult), reads=[p_.b, sn.b], writes=[b_.b])
                        fw.pool.op(lambda: nc.gpsimd.tensor_tensor(out=o3[:, :, 0:16], in0=a_[:], in1=b_[:], op=ALU.add),
                                   reads=[a_.b, b_.b], writes=[tmi.b])
                        fw.act.op(lambda: nc.scalar.copy(out=o3[:, :, 16:64], in_=x3[:, :, 16:64]),
                                  reads=[p_.b], writes=[tmi.b])
                    elif kind == "gate":
                        fw.act.op(lambda: nc.scalar.activation(out=dstv, in_=p_[:, lo:lo + wseg], func=AF.Sigmoid),
                                  reads=[p_.b], writes=[tmi.b])
                    else:
                        fw.act.op(lambda: nc.scalar.copy(out=dstv, in_=p_[:, lo:lo + wseg]),
                                  reads=[p_.b], writes=[tmi.b])
            for sidx, (kind, di) in enumerate(segs):
                if kind == "tm":
                    fw.sp.dma(tm_dst[i * 128:(i + 1) * 128, di:di + 256], tmi[:, sidx * 256:(sidx + 1) * 256],
                              reads=[tmi.b])
            st = stT[(i // 4) % 2]
            tcol = (i % 4) * 128
            pend = []
            for sidx, (kind, di) in enumerate(segs):
                if kind in ("rope", "fm", "gate"):
                    wseg = min(256, W - sidx * 256)
                    for hh in range(2):
                        wc = min(128, wseg - hh * 128)
                        if wc <= 0:
                            continue
                        pend.append((sidx * 256 + hh * 128, wc, di * 2 + hh))
            for j0 in range(0, len(pend), 8):
                grp = pend[j0:j0 + 8]
                p2 = ptr2[cnt["t"] % 2]
                cnt["t"] += 1
                for jj, (col, wc, ch) in enumerate(grp):
                    fw.pe.op(lambda jj=jj, col=col, wc=wc: nc.tensor.transpose(out=p2[0:wc, jj, :], in_=tmi[:, col:col + wc],
                                                                             identity=self.ident[:]),
                             reads=[tmi.b, self.ident.b], writes=[p2.b])
                ch0 = grp[0][2]
                contiguous = all(grp[jj][2] == ch0 + jj and grp[jj][1] == 128 for jj in range(len(grp)))
                if contiguous:
                    eng = fw.dve if (cnt["t"] % 2) else fw.act
                    if eng is fw.dve:
                        fw.dve.op(lambda: nc.vector.tensor_copy(out=st[:, ch0:ch0 + len(grp), tcol:tcol + 128],
                                                                in_=p2[:, 0:len(grp), :]), reads=[p2.b], writes=[st.b])
                    else:
                        fw.act.op(lambda: nc.scalar.copy(out=st[:, ch0:ch0 + len(grp), tcol:tcol + 128],
                                                         in_=p2[:, 0:len(grp), :]), reads=[p2.b], writes=[st.b])
                else:
                    for jj, (col, wc, ch) in enumerate(grp):
                        fw.dve.op(lambda jj=jj, wc=wc, ch=ch: nc.vector.tensor_copy(out=st[0:wc, ch, tcol:tcol + 128],
                                                                                  in_=p2[0:wc, jj, :]),
                                  reads=[p2.b], writes=[st.b])
            if i % 4 == 3:
                t0 = (i // 4) * 512
                fw.sp.dma(fm_view[:, :, t0:t0 + 512], st[:], reads=[st.b])
        fw.pop()

    def outproj(self, src, dst, oT_dram, w_out, g_post):
        nc, fw = self.nc, self.fw
        fw.push()
        wo = [fw.sb([128, D], BF16) for _ in range(8)]
        for c in range(8):
            fw.pool.dma(wo[c][:], w_out[c * 128:(c + 1) * 128, :], writes=[wo[c].b])
        gbc = fw.sb([128, D], F32)
        self.load_bc(gbc, g_post)
        oT = [fw.sb([128, 8, 512], BF16) for _ in range(2)]
        htiles = [fw.sb([128, D], F32) for _ in range(3)]
        tt_ = [fw.sb([128, D], F32) for _ in range(2)]
        ss2 = [fw.sb([128, 1], F32) for _ in range(2)]
        var2 = [fw.sb([128, 1], F32) for _ in range(2)]
        rs2 = [fw.sb([128, 1], F32) for _ in range(2)]
        py = [fw.ps([128, D], F32) for _ in range(2)]
        ov = oT_dram.rearrange("(c p) t -> p c t", p=128)
        for i in range(NT):
            o_ = oT[(i // 4) % 2]
            if i % 4 == 0:
                fw.sp.dma(o_[:], ov[:, :, i * 128:i * 128 + 512], writes=[o_.b])
            ht = htiles[i % 3]
            fw.sp.dma(ht[:], src[i * 128:(i + 1) * 128, :], writes=[ht.b])
            y = py[i % 2]
            tc = (i % 4) * 128
            for hh in range(2):
                for c in range(8):
                    fw.pe.op(lambda c=c: nc.tensor.matmul(y[:, hh * 512:(hh + 1) * 512], lhsT=o_[:, c, tc:tc + 128],
                                                          rhs=wo[c][:, hh * 512:(hh + 1) * 512],
                                                          start=(c == 0), stop=(c == 7)),
                             reads=[o_.b, wo[c].b], writes=[y.b])
            j = i % 2
            fw.act.op(lambda: nc.scalar.activation(out=self.junk[:], in_=y[:], func=AF.Square, accum_out=ss2[j][:]),
                      reads=[y.b], writes=[self.junk.b, ss2[j].b])
            fw.dve.op(lambda: nc.vector.tensor_scalar(out=var2[j][:], in0=ss2[j][:], scalar1=1.0 / D, scalar2=EPS,
                                                      op0=ALU.mult, op1=ALU.add), reads=[ss2[j].b], writes=[var2[j].b])
            fw.pool.op(lambda: nc.gpsimd.tensor_tensor(out=rs2[j][:], in0=var2[j][:], in1=self.mhalf[:], op=ALU.pow),
                       reads=[var2[j].b, self.mhalf.b], writes=[rs2[j].b])
            t = tt_[j]
            fw.dve.op(lambda: nc.vector.tensor_tensor(out=t[:], in0=y[:], in1=gbc[:], op=ALU.mult),
                      reads=[y.b, gbc.b], writes=[t.b])
            fw.dve.op(lambda: nc.vector.scalar_tensor_tensor(out=t[:], in0=t[:], scalar=rs2[j][:], in1=ht[:],
                                                             op0=ALU.mult, op1=ALU.add),
                      reads=[t.b, rs2[j].b, ht.b], writes=[t.b])
            fw.sp.dma(dst[i * 128:(i + 1) * 128, :], t[:], reads=[t.b])
        fw.pop()

    def att_state(self, n_s=3, n_pt=5, lag=2):
        fw = self.fw
        return {"i": 0, "ps_s": [fw.ps([128, 512], F32) for _ in range(n_s)],
                "pt": [fw.sb([128, 512], BF16) for _ in range(n_pt)], "pend": [], "lag": lag}

    def att_step(self, st, kT, qT, c0, c1, outs, start, rd, bias=None, masks=(), scale=0.125):
        nc, fw = self.nc, self.fw
        i = st["i"]
        st["i"] += 1
        S_ = st["ps_s"][i % len(st["ps_s"])]
        pt = st["pt"][i % len(st["pt"])]
        fw.pe.op(lambda: nc.tensor.matmul(S_[:, c0:c1], lhsT=kT, rhs=qT[:, c0:c1], start=True, stop=(bias is None)),
                 reads=rd, writes=[S_.b])
        if bias is not None:
            bl, br, b0, b1, brd = bias
            fw.pe.op(lambda: nc.tensor.matmul(S_[:, b0:b1], lhsT=bl, rhs=br[:, b0:b1], start=False, stop=True,
                                              skip_group_check=True), reads=brd, writes=[S_.b])
        fw.act.op(lambda: nc.scalar.activation(out=pt[:, c0:c1], in_=S_[:, c0:c1], func=AF.Exp, scale=scale),
                  reads=[S_.b], writes=[pt.b])
        for (m0, mw, mt, mc) in masks:
            fw.pool.op(lambda m0=m0, mw=mw, mt=mt, mc=mc: nc.gpsimd.tensor_tensor(
                out=pt[:, m0:m0 + mw], in0=pt[:, m0:m0 + mw], in1=mt[:, mc:mc + mw], op=ALU.mult),
                reads=[pt.b, mt.b], writes=[pt.b])

        def stage_c():
            for (po, lhsT, lrd) in outs:
                fw.pe.op(lambda po=po, lhsT=lhsT: nc.tensor.matmul(po[:, c0:c1], lhsT=lhsT, rhs=pt[:, c0:c1],
                                                                   start=start, stop=False, skip_group_check=True),
                         reads=[pt.b] + lrd, writes=[po.b])
        st["pend"].append(stage_c)
        while len(st["pend"]) > st["lag"]:
            st["pend"].pop(0)()

    def att_flush(self, st):
        while st["pend"]:
            st["pend"].pop(0)()

    def diff_attn(self, qkT, v_tm, oT_dram, lam_ap, subln_ap, lambda_init, tri_ap):
        nc, fw = self.nc, self.fw
        fw.push()
        tri = fw.sb([128, 128], BF16)
        fw.sp.dma(tri[:], tri_ap, writes=[tri.b])
        lam = fw.sb([128, 256], F32)
        fw.sp.dma(lam[:], lam_ap.rearrange("a d -> (a d)").partition_broadcast(128), writes=[lam.b])
        lj = fw.sb([128, 64], F32)
        s01 = fw.sb([128, 1], F32)
        s23 = fw.sb([128, 1], F32)
        nlam = fw.sb([128, 1], F32)
        fw.dve.op(lambda: nc.vector.scalar_tensor_tensor(out=lj[:], in0=lam[:, 0:64], scalar=1.0, in1=lam[:, 64:128],
                                                         op0=ALU.mult, op1=ALU.mult, accum_out=s01[:]),
                  reads=[lam.b], writes=[lj.b, s01.b])
        fw.dve.op(lambda: nc.vector.scalar_tensor_tensor(out=lj[:], in0=lam[:, 128:192], scalar=1.0, in1=lam[:, 192:256],
                                                         op0=ALU.mult, op1=ALU.mult, accum_out=s23[:]),
                  reads=[lam.b], writes=[lj.b, s23.b])
        fw.act.op(lambda: nc.scalar.activation(out=s01[:], in_=s01[:], func=AF.Exp), reads=[s01.b], writes=[s01.b])
        fw.act.op(lambda: nc.scalar.activation(out=s23[:], in_=s23[:], func=AF.Exp), reads=[s23.b], writes=[s23.b])
        fw.dve.op(lambda: nc.vector.tensor_tensor(out=nlam[:], in0=s23[:], in1=s01[:], op=ALU.subtract),
                  reads=[s01.b, s23.b], writes=[nlam.b])
        fw.dve.op(lambda: nc.vector.tensor_scalar(out=nlam[:], in0=nlam[:], scalar1=-float(lambda_init), scalar2=None,
                                                  op0=ALU.add), reads=[nlam.b], writes=[nlam.b])
        gs = fw.sb([128, 1], F32)
        fw.sp.dma(gs[:], subln_ap.rearrange("(p o) -> p o", o=1), writes=[gs.b])
        fw.dve.op(lambda: nc.vector.tensor_scalar(out=gs[:], in0=gs[:], scalar1=float(1.0 - lambda_init), scalar2=None,
                                                  op0=ALU.mult), reads=[gs.b], writes=[gs.b])
        qT = [fw.sb([128, S], BF16) for _ in range(2)]
        kT = [fw.sb([128, S], BF16) for _ in range(2)]
        vv = fw.sb([128, NT, D], BF16)
        v_view = v_tm.rearrange("(t p) c -> p t c", p=128)
        for t0 in range(0, NT, 8):
            fw.sp.dma(vv[:, t0:t0 + 8, :], v_view[:, t0:t0 + 8, :], writes=[vv.b])
        st = self.att_state()
        pO = [fw.ps([128, 512], F32) for _ in range(2)]
        pS = [fw.ps([128, 512], F32) for _ in range(2)]
        pQ = pO[0] if False else fw.ps([128, 512], F32)
        r0 = fw.sb([128, 512], F32)
        r1 = fw.sb([128, 512], F32)
        a0 = fw.sb([128, 512], F32)
        a1 = fw.sb([128, 512], F32)
        sq = fw.sb([128, 512], BF16)
        rst = fw.sb([128, 512], F32)
        ob = [fw.sb([128, 512], BF16) for _ in range(2)]
        import os
        nhead = int(os.environ.get('NHEAD', 8))
        for h in range(int(os.environ.get('HSTART', 0)), nhead):
            q_, k_ = qT[h % 2], kT[h % 2]
            fw.sp.dma(q_[:], qkT[h * 128:(h + 1) * 128, :], writes=[q_.b])
            fw.sp.dma(k_[:], qkT[1024 + h * 128:1024 + (h + 1) * 128, :], writes=[k_.b])
            for qg in range(8):
                for c in range(2):
                    nk = 4 * qg + 4
                    for kt in range(nk):
                        v = kt - 4 * qg
                        c0 = 128 * v if v > 0 else 0
                        self.att_step(st, k_[c * 64:(c + 1) * 64, kt * 128:(kt + 1) * 128],
                                      q_[c * 64:(c + 1) * 64, qg * 512:(qg + 1) * 512], c0, 512,
                                      [(pO[c], vv[:, kt, h * 128:(h + 1) * 128], [vv.b]),
                                       (pS[c], self.ones_bf[:], [self.ones_bf.b])],
                                      start=(kt == 0), rd=[q_.b, k_.b],
                                      masks=([(c0, 128, tri, 0)] if v >= 0 else ()))
                self.att_flush(st)
                fw.dve.op(lambda: nc.vector.reciprocal(out=r0[:], in_=pS[0][:]), reads=[pS[0].b], writes=[r0.b])
                fw.dve.op(lambda: nc.vector.reciprocal(out=r1[:], in_=pS[1][:]), reads=[pS[1].b], writes=[r1.b])
                fw.dve.op(lambda: nc.vector.tensor_tensor(out=a0[:], in0=pO[0][:], in1=r0[:], op=ALU.mult),
                          reads=[pO[0].b, r0.b], writes=[a0.b])
                fw.dve.op(lambda: nc.vector.tensor_tensor(out=a1[:], in0=pO[1][:], in1=r1[:], op=ALU.mult),
                          reads=[pO[1].b, r1.b], writes=[a1.b])
                fw.dve.op(lambda: nc.vector.scalar_tensor_tensor(out=a0[:], in0=a1[:], scalar=nlam[:], in1=a0[:],
                                                                 op0=ALU.mult, op1=ALU.add),
                          reads=[a0.b, a1.b, nlam.b], writes=[a0.b])
                fw.pool.op(lambda: nc.gpsimd.tensor_tensor(out=sq[:], in0=a0[:], in1=a0[:], op=ALU.mult),
                           reads=[a0.b], writes=[sq.b])
                fw.pe.op(lambda: nc.tensor.matmul(pQ[:], lhsT=self.ones_bf[:], rhs=sq[:], start=True, stop=True),
                         reads=[sq.b, self.ones_bf.b], writes=[pQ.b])
                fw.dve.op(lambda: nc.vector.tensor_scalar(out=rst[:], in0=pQ[:], scalar1=1.0 / 128, scalar2=1e-5,
                                                          op0=ALU.mult, op1=ALU.add), reads=[pQ.b], writes=[rst.b])
                fw.pool.op(lambda: nc.gpsimd.tensor_tensor(out=rst[:], in0=rst[:],
                                                           in1=self.mhalf[:, 0:1].to_broadcast([128, 512]), op=ALU.pow),
                           reads=[rst.b, self.mhalf.b], writes=[rst.b])
                o_ = ob[(h * 8 + qg) % 2]
                fw.dve.op(lambda: nc.vector.scalar_tensor_tensor(out=o_[:], in0=a0[:], scalar=gs[:], in1=rst[:],
                                                                 op0=ALU.mult, op1=ALU.mult),
                          reads=[a0.b, gs.b, rst.b], writes=[o_.b])
                fw.sp.dma(oT_dram[h * 128:(h + 1) * 128, qg * 512:(qg + 1) * 512], o_[:], reads=[o_.b])
            fw.barrier()
        fw.pop()


    def moba_attn(self, qkT, v_tm, oT_dram, tri_ap, vb_ap, e16_ap):
        nc, fw = self.nc, self.fw
        fw.push()
        tri = fw.sb([128, 128], BF16)
        fw.sp.dma(tri[:], tri_ap, writes=[tri.b])
        vb = fw.sb([128, NT, 16], F32)
        fw.sp.dma(vb[:], vb_ap.rearrange("p (t b) -> p t b", b=16), writes=[vb.b])
        e16 = fw.sb([16, 16, 128], BF16)
        fw.sp.dma(e16[:], e16_ap, writes=[e16.b])
        vv = fw.sb([128, NT, D], BF16)
        v_view = v_tm.rearrange("(t p) c -> p t c", p=128)
        for t0 in range(0, NT, 8):
            fw.sp.dma(vv[:, t0:t0 + 8, :], v_view[:, t0:t0 + 8, :], writes=[vv.b])
        qT = [fw.sb([128, S], BF16) for _ in range(2)]
        kT = [fw.sb([128, S], BF16) for _ in range(2)]
        st = self.att_state()
        pO = fw.ps([128, 512], F32)
        pS = fw.ps([128, 512], F32)
        pG = fw.ps([128, NT, 16], F32)
        pT = fw.ps([128, 4, 128], BF16)
        kmf = fw.sb([128, 16], F32)
        kmb = fw.sb([128, 16], BF16)
        gsb = fw.sb([128, NT, 16], F32)
        m8 = fw.sb([128, NT, 8], F32)
        selb = fw.sb([128, NT, 16], F32)
        biasb = [fw.sb([128, NT, 16], BF16) for _ in range(2)]
        sbT = [fw.sb([16, 512], BF16) for _ in range(2)]
        rr = fw.sb([128, 512], F32)
        ob = [fw.sb([128, 512], BF16) for _ in range(2)]
        cnt = 0
        for j in range(8):
            q_, k_ = qT[j % 2], kT[j % 2]
            fw.sp.dma(q_[:], qkT[j * 128:(j + 1) * 128, :], writes=[q_.b])
            fw.sp.dma(k_[:], qkT[1024 + j * 128:1024 + (j + 1) * 128, :], writes=[k_.b])
            for hh in range(2):
                b0_, b1_ = hh * 64, hh * 64 + 64
                bb = biasb[hh]
                fw.dve.op(lambda: nc.vector.tensor_reduce(out=kmf[b0_:b1_, :],
                                                          in_=k_[b0_:b1_, :].rearrange("p (b l) -> p b l", l=256),
                                                          axis=AX.X, op=ALU.add), reads=[k_.b], writes=[kmf.b])
                fw.dve.op(lambda: nc.vector.tensor_scalar(out=kmb[b0_:b1_, :], in0=kmf[b0_:b1_, :], scalar1=1.0 / 256,
                                                          scalar2=None, op0=ALU.mult), reads=[kmf.b], writes=[kmb.b])
                for qt in range(NT):
                    fw.pe.op(lambda qt=qt: nc.tensor.matmul(pG[:, qt, :], lhsT=q_[b0_:b1_, qt * 128:(qt + 1) * 128],
                                                            rhs=kmb[b0_:b1_, :], start=True, stop=True,
                                                            skip_group_check=True),
                             reads=[q_.b, kmb.b], writes=[pG.b])
                fw.dve.op(lambda: nc.vector.tensor_tensor(out=gsb[:], in0=pG[:], in1=vb[:], op=ALU.add),
                          reads=[pG.b, vb.b], writes=[gsb.b])
                for qt in range(NT):
                    fw.dve.op(lambda qt=qt: nc.vector.max(out=m8[:, qt, :], in_=gsb[:, qt, :]),
                              reads=[gsb.b], writes=[m8.b])
                fw.dve.op(lambda: nc.vector.tensor_tensor(out=selb[:], in0=gsb[:],
                                                          in1=m8[:, :, 2:3].to_broadcast([128, NT, 16]), op=ALU.is_ge),
                          reads=[gsb.b, m8.b], writes=[selb.b])
                fw.dve.op(lambda: nc.vector.tensor_scalar(out=bb[:], in0=selb[:], scalar1=-NEG, scalar2=NEG,
                                                          op0=ALU.mult, op1=ALU.add), reads=[selb.b], writes=[bb.b])
            for qg in range(8):
                o_ = ob[(j * 8 + qg) % 2]
                for hh in range(2):
                    b0_, b1_ = hh * 64, hh * 64 + 64
                    bb = biasb[hh]
                    sT = sbT[cnt % 2]
                    cnt += 1
                    for t in range(4):
                        fw.pe.op(lambda t=t: nc.tensor.transpose(out=pT[0:16, t, :], in_=bb[:, 4 * qg + t, :],
                                                                 identity=self.ident[:]),
                                 reads=[bb.b, self.ident.b], writes=[pT.b])
                    fw.dve.op(lambda: nc.vector.tensor_copy(out=sT[:], in_=pT[0:16, :, :].rearrange("p a b -> p (a b)")),
                              reads=[pT.b], writes=[sT.b])
                    for kt in range(4 * qg + 4):
                        v = kt - 4 * qg
                        blk = kt // 2
                        bias = None
                        masks = ()
                        c0 = 0
                        if v < 0:
                            bias = (e16[:, blk, :], sT, 0, 512, [sT.b, e16.b])
                        else:
                            c0 = 128 * v
                            masks = [(c0, 128, tri, 0)]
                            if v < 2:
                                bias = (e16[:, blk, :], sT, 256, 512, [sT.b, e16.b])
                        self.att_step(st, k_[b0_:b1_, kt * 128:(kt + 1) * 128], q_[b0_:b1_, qg * 512:(qg + 1) * 512],
                                      c0, 512, [(pO, vv[:, kt, j * 128:(j + 1) * 128], [vv.b]),
                                                (pS, self.ones_bf[:], [self.ones_bf.b])],
                                      start=(kt == 0), rd=[q_.b, k_.b], bias=bias, masks=masks)
                    self.att_flush(st)
                    fw.dve.op(lambda: nc.vector.reciprocal(out=rr[b0_:b1_, :], in_=pS[b0_:b1_, :]),
                              reads=[pS.b], writes=[rr.b])
                    fw.dve.op(lambda: nc.vector.tensor_tensor(out=o_[b0_:b1_, :], in0=pO[b0_:b1_, :], in1=rr[b0_:b1_, :],
                                                              op=ALU.mult), reads=[pO.b, rr.b], writes=[o_.b])
                fw.sp.dma(oT_dram[j * 128:(j + 1) * 128, qg * 512:(qg + 1) * 512], o_[:], reads=[o_.b])
            fw.barrier()
        fw.pop()


    def nsa_attn(self, FM, TM, oT_dram, pe_ap, w1_ap, w2_ap, C):
        nc, fw = self.nc, self.fw
        io = self.io
        fw.push()
        tri = fw.sb([128, 128], BF16)
        fw.sp.dma(tri[:], C["tri"], writes=[tri.b])
        wm = fw.sb([128, 128], BF16)
        fw.sp.dma(wm[:], C["nsa_wm"], writes=[wm.b])
        ttab = fw.sb([128, 3072], BF16)
        fw.sp.dma(ttab[:], C["nsa_ttab"], writes=[ttab.b])
        e64 = fw.sb([128, NT, 128], BF16)
        fw.sp.dma(e64[0:64, :, :], C["nsa_e64"], writes=[e64.b])
        fw.sp.dma(e64[64:128, :, :], C["nsa_e64"], writes=[e64.b])
        selg = fw.sb([48, 48, 128], BF16)
        fw.sp.dma(selg[:], C["nsa_selg"], writes=[selg.b])
        ta = fw.sb([128, NT, 64], F32)
        tb = fw.sb([128, NT, 64], F32)
        fw.sp.dma(ta[:], C["nsa_ta"].rearrange("p (t b) -> p t b", b=64), writes=[ta.b])
        fw.sp.dma(tb[:], C["nsa_tb"].rearrange("p (t b) -> p t b", b=64), writes=[tb.b])
        id64 = fw.sb([128, 64], BF16)
        fw.sp.dma(id64[:], C["nsa_id64"], writes=[id64.b])
        gT = fw.sb([48, S], BF16)
        fw.sp.dma(gT[:], FM[2048:2096, :], writes=[gT.b])
        kcmpT = [fw.sb([128, 256], BF16) for _ in range(4)]
        vaug = [[fw.sb([128, 2, 128], BF16) for _ in range(2)] for _ in range(4)]
        ovf = fw.sb([128, 2, 64], BF16)
        fw.sp.dma(ovf[:], C["nsa_ov"].rearrange("(a p) b -> p a b", p=128), writes=[ovf.b])
        for g in range(4):
            fw.dve.op(lambda g=g: nc.vector.memset(kcmpT[g][:], 0.0), writes=[kcmpT[g].b])
            for vr in range(2):
                va = vaug[g][vr]
                fw.dve.op(lambda va=va: nc.vector.memset(va[:], 0.0), writes=[va.b])
                oc = 64 if vr == 0 else 0
                fw.dve.op(lambda va=va, oc=oc: nc.vector.tensor_copy(out=va[:, :, oc:oc + 64], in_=ovf[:]),
                          reads=[ovf.b], writes=[va.b])
        import os
        fw.push()
        _CS = os.environ.get("NSA_CSKIP", "")
        xT = fw.sb([128, 2, 2, S + 16], BF16)
        fw.dve.op(lambda: nc.vector.memset(xT[:, :, :, S:S + 16], 0.0), writes=[xT.b])
        fw.sp.dma(xT[:, 0, :, 0:S], FM[1024:1280, :].rearrange("(c p) t -> p c t", p=128), writes=[xT.b])
        fw.sp.dma(xT[:, 1, :, 0:S], FM[1280:1536, :].rearrange("(c p) t -> p c t", p=128), writes=[xT.b])
        w1sb = [fw.sb([128, 32, 256], BF16) for _ in range(2)]
        w2f = [fw.sb([128, 2, 64], BF16) for _ in range(2)]
        w2d = [fw.sb([128, 2, 128], BF16) for _ in range(2)]
        pef = fw.sb([128, 2, 32], F32)
        peb = fw.sb([128, 2, 32], BF16)
        for i in range(2):
            for hb in range(2):
                fw.pool.dma(w1sb[i][hb * 64:(hb + 1) * 64, :, :], w1_ap[i].rearrange("(t d) n -> d t n", d=64),
                            writes=[w1sb[i].b])
                fw.sp.dma(pef[hb * 64:(hb + 1) * 64, i, :], pe_ap[i].rearrange("t d -> d t"), writes=[pef.b],
                          allow_slow_non_contiguous=True)
            fw.pool.dma(w2f[i][:], w2_ap[i].rearrange("(a p) d -> p a d", p=128), writes=[w2f[i].b])
            for hb in range(2):
                fw.dve.op(lambda i=i, hb=hb: nc.vector.tensor_copy(out=w2d[i][:, :, hb * 64:(hb + 1) * 64], in_=w2f[i][:]),
                          reads=[w2f[i].b], writes=[w2d[i].b])
        fw.dve.op(lambda: nc.vector.tensor_copy(out=peb[:], in_=pef[:]), reads=[pef.b], writes=[peb.b])
        pb = fw.ps([128, 4], F32)
        b1 = fw.sb([128, 4], F32)
        for i in (range(2) if "b1" not in _CS else ()):
            for a in range(2):
                for t in range(32):
                    fw.pe.op(lambda i=i, a=a, t=t: nc.tensor.matmul(pb[:, 2 * i + a:2 * i + a + 1],
                                                                    lhsT=w1sb[i][0:64, t, a * 128:(a + 1) * 128],
                                                                    rhs=peb[0:64, i, t:t + 1], start=(t == 0 and i == 0 and a == 0),
                                                                    stop=False, skip_group_check=True),
                             reads=[w1sb[i].b, peb.b], writes=[pb.b])
        fw.dve.op(lambda: nc.vector.tensor_copy(out=b1[:], in_=pb[:]), reads=[pb.b], writes=[b1.b])
        ph = [fw.ps([128, 256], F32) for _ in range(2)]
        pk = fw.ps([128, 256], F32)
        pv = fw.ps([128, 2, 64], F32)
        hsb = [fw.sb([128, 2, 256], BF16) for _ in range(2)]
        cc_ = 0
        for i in (range(2) if "mlp" not in _CS else ()):
            for g in range(4):
                c, base = g // 2, (g % 2) * 64
                xv = xT[base:base + 64, i, c, :].rearrange("p (n s) -> p n s", s=16)
                hs = hsb[(i * 4 + g) % 2]
                fw.dve.op(lambda hs=hs: nc.vector.memset(hs[:], 0.0), writes=[hs.b])
                for a in range(2):
                    p_ = ph[cc_ % 2]
                    cc_ += 1
                    for t in range(32):
                        rhs = xv[:, 0:256, t] if t < 16 else xv[:, 1:257, t - 16]
                        fw.pe.op(lambda t=t, rhs=rhs, a=a: nc.tensor.matmul(p_[:, 0:256],
                                                                            lhsT=w1sb[i][base:base + 64, t, a * 128:(a + 1) * 128],
                                                                            rhs=rhs, start=(t == 0), stop=(t == 31)),
                                 reads=[xT.b, w1sb[i].b], writes=[p_.b])
                    fw.act.op(lambda a=a, p_=p_: nc.scalar.activation(out=hs[:, a, 0:256], in_=p_[:, 0:256], func=AF.Silu,
                                                                      bias=b1[:, 2 * i + a:2 * i + a + 1]),
                              reads=[p_.b, b1.b], writes=[hs.b])
                if i == 0:
                    for a in range(2):
                        fw.pe.op(lambda a=a: nc.tensor.matmul(pk[:, 0:256], lhsT=w2d[0][:, a, :], rhs=hs[:, a, 0:256],
                                                              start=(a == 0), stop=(a == 1)),
                                 reads=[w2d[0].b, hs.b], writes=[pk.b])
                    fw.dve.op(lambda g=g: nc.vector.tensor_copy(out=kcmpT[g][:, 0:256], in_=pk[:, 0:256]),
                              reads=[pk.b], writes=[kcmpT[g].b])
                else:
                    for ntile in range(2):
                        for a in range(2):
                            fw.pe.op(lambda a=a, ntile=ntile: nc.tensor.matmul(pv[:, ntile, :],
                                                                               lhsT=hs[:, a, ntile * 128:(ntile + 1) * 128],
                                                                               rhs=w2f[1][:, a, :], start=(a == 0), stop=(a == 1),
                                                                               skip_group_check=True),
                                     reads=[w2f[1].b, hs.b], writes=[pv.b])
                    for vr in range(2):
                        va = vaug[g][vr]
                        vc0 = 0 if vr == 0 else 64
                        fw.dve.op(lambda va=va, vc0=vc0: nc.vector.tensor_copy(out=va[:, :, vc0:vc0 + 64], in_=pv[:]),
                                  reads=[pv.b], writes=[va.b])
        fw.pop()
        st = self.att_state()
        pO = fw.ps([128, 512], F32)
        pS = fw.ps([128, 512], F32)
        pGt = fw.ps([128, 512], F32)
        pI = fw.ps([128, 512], F32)
        pTb = fw.ps([128, 4, 128], BF16)
        qc = fw.sb([128, 2, S], BF16)
        ksd = fw.sb([128, S], BF16)
        kwd = fw.sb([128, S], BF16)
        vsd = fw.sb([128, NT, 128], BF16)
        vwd = fw.sb([128, NT, 128], BF16)
        stg = [fw.sb([128, 8, 512], BF16) for _ in range(2)]
        acc = [fw.sb([128, 512], F32) for _ in range(4)]
        impE = fw.sb([128, 512], F32)
        impH = fw.sb([128, 512], BF16)
        impL = fw.sb([128, 512], BF16)
        xs = fw.sb([128, 512], F32)
        rr = fw.sb([128, 512], F32)
        t1 = fw.sb([128, 64], F32)
        t2 = fw.sb([128, 64], F32)
        m8a = fw.sb([128, 8], F32)
        m8b = fw.sb([128, 8], F32)
        selb = fw.sb([128, 64], F32)
        bb = fw.sb([128, 128], BF16)
        sbT = fw.sb([128, 512], BF16)
        ob = [fw.sb([128, 512], BF16) for _ in range(4)]
        tm_view = TM.rearrange("(t p) c -> p t c", p=128)
        nstg = 0

        def gate_mul(r, h, br, base, qg, first):
            fw.pe.op(lambda: nc.tensor.matmul(pGt[:], lhsT=selg[:, h * 3 + br, :], rhs=gT[:, qg * 512:(qg + 1) * 512],
                                              start=True, stop=True), reads=[selg.b, gT.b], writes=[pGt.b])
            if first:
                fw.dve.op(lambda: nc.vector.tensor_tensor(out=acc[r][base:base + 64, :], in0=xs[base:base + 64, :],
                                                          in1=pGt[base:base + 64, :], op=ALU.mult),
                          reads=[xs.b, pGt.b], writes=[acc[r].b])
            else:
                fw.dve.op(lambda: nc.vector.tensor_tensor(out=xs[base:base + 64, :], in0=xs[base:base + 64, :],
                                                          in1=pGt[base:base + 64, :], op=ALU.mult),
                          reads=[xs.b, pGt.b], writes=[xs.b])
                fw.pool.op(lambda: nc.gpsimd.tensor_tensor(out=acc[r][base:base + 64, :], in0=acc[r][base:base + 64, :],
                                                           in1=xs[base:base + 64, :], op=ALU.add),
                           reads=[xs.b, acc[r].b], writes=[acc[r].b])

        def normalize(lo, hi, guard):
            if guard:
                fw.dve.op(lambda: nc.vector.tensor_scalar(out=rr[lo:hi, :], in0=pS[lo:hi, :], scalar1=1e-30, scalar2=None,
                                                          op0=ALU.max), reads=[pS.b], writes=[rr.b])
                fw.dve.op(lambda: nc.vector.reciprocal(out=rr[lo:hi, :], in_=rr[lo:hi, :]), reads=[rr.b], writes=[rr.b])
            else:
                fw.dve.op(lambda: nc.vector.reciprocal(out=rr[lo:hi, :], in_=pS[lo:hi, :]), reads=[pS.b], writes=[rr.b])
            fw.dve.op(lambda: nc.vector.tensor_tensor(out=xs[lo:hi, :], in0=pO[lo:hi, :], in1=rr[lo:hi, :], op=ALU.mult),
                      reads=[pO.b, rr.b], writes=[xs.b])

        import os
        _G = [int(v) for v in os.environ.get("NSA_G", "0,1,2,3").split(",")]
        _QG = [int(v) for v in os.environ.get("NSA_QG", "0,1,2,3,4,5,6,7").split(",")]
        _SKIP = set(os.environ.get("NSA_SKIP", "").split(","))
        for g in _G:
            kc_, kb_ = 12 + g // 2, (g % 2) * 64
            fw.sp.dma(qc[:], FM[g * 256:(g + 1) * 256, :].rearrange("(c p) t -> p c t", p=128), writes=[qc.b])
            for hb in range(2):
                fw.sp.dma(ksd[hb * 64:(hb + 1) * 64, :], FM[1536 + g * 64:1536 + (g + 1) * 64, :], writes=[ksd.b])
                fw.sp.dma(kwd[hb * 64:(hb + 1) * 64, :], FM[1792 + g * 64:1792 + (g + 1) * 64, :], writes=[kwd.b])
            for t0 in range(0, NT, 8):
                sg_ = stg[nstg % 2]
                nstg += 1
                fw.sp.dma(sg_[:], tm_view[:, t0:t0 + 8, 0:512], writes=[sg_.b])
                for hb in range(2):
                    fw.pool.op(lambda sg_=sg_, hb=hb, t0=t0: nc.gpsimd.tensor_copy(
                        out=vsd[:, t0:t0 + 8, hb * 64:(hb + 1) * 64], in_=sg_[:, :, g * 64:(g + 1) * 64]),
                        reads=[sg_.b], writes=[vsd.b])
                    fw.pool.op(lambda sg_=sg_, hb=hb, t0=t0: nc.gpsimd.tensor_copy(
                        out=vwd[:, t0:t0 + 8, hb * 64:(hb + 1) * 64], in_=sg_[:, :, 256 + g * 64:256 + (g + 1) * 64]),
                        reads=[sg_.b], writes=[vwd.b])
            for qg in _QG:
                for r in (range(4) if "cmp" not in _SKIP else ()):
                    h = 4 * g + r
                    base = (r % 2) * 64
                    oth = 64 - base
                    q_ = qc[base:base + 64, r // 2, qg * 512:(qg + 1) * 512]
                    first = True
                    for ntile in range(2):
                        s_ = 512 * qg - 2048 * ntile
                        if s_ < 0:
                            continue
                        masks = [(0, 512, ttab, s_)] if s_ < 2560 else ()
                        self.att_step(st, kcmpT[g][base:base + 64, ntile * 128:(ntile + 1) * 128], q_, 0, 512,
                                      [(pO, vaug[g][r % 2][:, ntile, :], [vaug[g][r % 2].b]),
                                       (pS, self.ones_bf[:], [self.ones_bf.b])],
                                      start=first, rd=[qc.b, kcmpT[g].b], masks=masks)
                        first = False
                    self.att_flush(st)
                    normalize(0, 128, True)
                    if r < 2:
                        fw.pool.op(lambda oth=oth: nc.gpsimd.tensor_copy(out=impE[oth:oth + 64, :], in_=xs[oth:oth + 64, :]),
                                   reads=[xs.b], writes=[impE.b])
                    else:
                        fw.pool.op(lambda oth=oth: nc.gpsimd.tensor_tensor(out=impE[oth:oth + 64, :], in0=impE[oth:oth + 64, :],
                                                                           in1=xs[oth:oth + 64, :], op=ALU.add),
                                   reads=[xs.b, impE.b], writes=[impE.b])
                    gate_mul(r, h, 0, base, qg, True)
                for r in (range(4) if "win" not in _SKIP else ()):
                    h = 4 * g + r
                    base = (r % 2) * 64
                    q_ = qc[base:base + 64, r // 2, qg * 512:(qg + 1) * 512]
                    first = True
                    for m in range(-4, 4):
                        kt = 4 * qg + m
                        if kt < 0:
                            continue
                        if m < 0:
                            c0, c1 = 0, 128 * (m + 5)
                            masks = [(c1 - 128, 128, wm, 0)]
                        else:
                            c0, c1 = 128 * m, 512
                            masks = [(c0, 128, tri, 0)]
                        self.att_step(st, kwd[base:base + 64, kt * 128:(kt + 1) * 128], q_, c0, c1,
                                      [(pO, vwd[:, kt, :], [vwd.b]), (pS, self.ones_bf[:], [self.ones_bf.b])],
                                      start=first, rd=[qc.b, kwd.b], masks=masks)
                        first = False
                    self.att_flush(st)
                    normalize(base, base + 64, False)
                    gate_mul(r, h, 2, base, qg, False)
                for t in (range(4) if "topk" not in _SKIP else ()):
                    qt = 4 * qg + t
                    pi = pI[:, t * 64:(t + 1) * 64]
                    if t == 0:
                        fw.dve.op(lambda: nc.vector.tensor_copy(out=impH[:], in_=impE[:]), reads=[impE.b], writes=[impH.b])
                        fw.dve.op(lambda: nc.vector.tensor_tensor(out=impL[:], in0=impE[:], in1=impH[:], op=ALU.subtract),
                                  reads=[impE.b, impH.b], writes=[impL.b])
                    for k4, src_ in enumerate((impH, impL)):
                        fw.pe.op(lambda src_=src_, k4=k4, t=t, pi=pi: nc.tensor.matmul(
                            pi, lhsT=src_[:, t * 128:(t + 1) * 128], rhs=id64[:, :],
                            start=(k4 == 0), stop=(k4 == 1), skip_group_check=True),
                            reads=[src_.b, id64.b], writes=[pI.b])
                    _LV = int(os.environ.get("TOPK_LEVEL", 9))
                    if _LV < 2:
                        continue
                    fw.dve.op(lambda pi=pi, qt=qt: nc.vector.tensor_tensor(out=t1[:], in0=pi, in1=ta[:, qt, :], op=ALU.mult),
                              reads=[pI.b, ta.b], writes=[t1.b])

                    fw.dve.op(lambda qt=qt: nc.vector.tensor_tensor(out=t1[:], in0=t1[:], in1=tb[:, qt, :], op=ALU.add),
                              reads=[t1.b, tb.b], writes=[t1.b])
                    if _LV < 3:
                        continue
                    fw.dve.op(lambda: nc.vector.max(out=m8a[:], in_=t1[:]), reads=[t1.b], writes=[m8a.b])
                    if _LV < 4:
                        continue
                    fw.dve.op(lambda: nc.vector.tensor_tensor(out=t2[:], in0=t1[:], in1=m8a[:, 7:8].to_broadcast([128, 64]),
                                                              op=ALU.is_ge), reads=[t1.b, m8a.b], writes=[t2.b])
                    fw.dve.op(lambda: nc.vector.scalar_tensor_tensor(out=t2[:], in0=t2[:], scalar=-1.0e10, in1=t1[:],
                                                                     op0=ALU.mult, op1=ALU.add),
                              reads=[t1.b, t2.b], writes=[t2.b])
                    fw.dve.op(lambda: nc.vector.max(out=m8b[:], in_=t2[:]), reads=[t2.b], writes=[m8b.b])
                    fw.dve.op(lambda: nc.vector.tensor_tensor(out=selb[:], in0=t1[:], in1=m8b[:, 7:8].to_broadcast([128, 64]),
                                                              op=ALU.is_ge), reads=[t1.b, m8b.b], writes=[selb.b])
                    fw.dve.op(lambda: nc.vector.tensor_scalar(out=bb[:, 0:64], in0=selb[:], scalar1=-NEG, scalar2=NEG,
                                                              op0=ALU.mult, op1=ALU.add), reads=[selb.b], writes=[bb.b])
                    fw.dve.op(lambda: nc.vector.tensor_scalar(out=bb[:, 64:128], in0=selb[:], scalar1=-NEG, scalar2=NEG,
                                                              op0=ALU.mult, op1=ALU.add), reads=[selb.b], writes=[bb.b])
                    if _LV < 5:
                        continue
                    fw.pe.op(lambda t=t: nc.tensor.transpose(out=pTb[:, t, :], in_=bb[:], identity=self.ident[:]),
                             reads=[bb.b, self.ident.b], writes=[pTb.b])
                fw.dve.op(lambda: nc.vector.tensor_copy(out=sbT[:], in_=pTb[:, :, :].rearrange("p a b -> p (a b)")),
                          reads=[pTb.b], writes=[sbT.b])
                for r in (range(4) if "sel" not in _SKIP else ()):
                    h = 4 * g + r
                    base = (r % 2) * 64
                    q_ = qc[base:base + 64, r // 2, qg * 512:(qg + 1) * 512]
                    for kt in range(4 * qg + 4):
                        v = kt - 4 * qg
                        c0 = 128 * v if v > 0 else 0
                        masks = [(c0, 128, tri, 0)] if v >= 0 else ()
                        self.att_step(st, ksd[base:base + 64, kt * 128:(kt + 1) * 128], q_, c0, 512,
                                      [(pO, vsd[:, kt, :], [vsd.b]), (pS, self.ones_bf[:], [self.ones_bf.b])],
                                      start=(kt == 0), rd=[qc.b, ksd.b],
                                      bias=(e64[base:base + 64, kt, :], sbT[base:base + 64, :], c0, 512, [sbT.b, e64.b]),
                                      masks=masks)
                    self.att_flush(st)
                    normalize(base, base + 64, False)
                    gate_mul(r, h, 1, base, qg, False)
                for cc2 in range(2):
                    o_ = ob[(qg * 2 + cc2) % 4]
                    for hb in range(2):
                        r = cc2 * 2 + hb
                        fw.act.op(lambda r=r, hb=hb, o_=o_: nc.scalar.copy(out=o_[hb * 64:(hb + 1) * 64, :],
                                                                          in_=acc[r][hb * 64:(hb + 1) * 64, :]),
                                  reads=[acc[r].b], writes=[o_.b])
                    fw.sp.dma(oT_dram[(2 * g + cc2) * 128:(2 * g + cc2 + 1) * 128, qg * 512:(qg + 1) * 512], o_[:],
                              reads=[o_.b])
            fw.barrier()
        fw.pop()


def host_consts():
    pos = np.arange(S, dtype=np.float32)
    half = 8
    inv_freq = (np.float32(500000.0) ** (-np.arange(half, dtype=np.float32) / half)).astype(np.float32)
    ang = (pos[:, None] * inv_freq[None, :]).astype(np.float32)
    cos = np.cos(ang).astype(np.float32)
    sin = np.sin(ang).astype(np.float32)
    c = {}
    c["rope_cc"] = np.concatenate([cos, cos], axis=1)
    c["rope_ss"] = np.concatenate([-sin, sin], axis=1)
    p = np.arange(128)
    c["tri"] = (p[:, None] <= p[None, :]).astype(np.float32).astype(ml_dtypes.bfloat16)
    vb = np.zeros((128, NT, 16), np.float32)
    for qt in range(NT):
        vb[:, qt, qt // 2:] = -1e30
    c["moba_vb"] = vb.reshape(128, NT * 16)
    e16 = np.zeros((16, 16, 128), np.float32)
    for b in range(16):
        e16[b, b, :] = 1.0
    c["moba_e16"] = e16.astype(ml_dtypes.bfloat16)
    c["nsa_wm"] = (p[None, :] < p[:, None]).astype(np.float32).astype(ml_dtypes.bfloat16)
    jj = np.arange(3072)
    c["nsa_ttab"] = (jj[None, :] >= 16 * p[:, None] + 31).astype(np.float32).astype(ml_dtypes.bfloat16)
    e64 = np.zeros((64, NT, 128), np.float32)
    for kt in range(NT):
        e64[2 * kt, kt, 0:64] = 1.0
        e64[2 * kt + 1, kt, 64:128] = 1.0
    c["nsa_e64"] = e64.astype(ml_dtypes.bfloat16)
    sg = np.zeros((48, 48, 128), np.float32)
    for rr_ in range(48):
        sg[rr_, rr_, :] = 1.0
    c["nsa_selg"] = sg.astype(ml_dtypes.bfloat16)
    ta = np.zeros((128, NT, 64), np.float32)
    tb = np.zeros((128, NT, 64), np.float32)
    blk = np.arange(64)
    for qt in range(NT):
        for pp in range(128):
            qb = (qt * 128 + pp) // 64
            valid = blk <= qb
            a_ = valid.astype(np.float32)
            b_ = np.where(valid, 0.0, -1e30).astype(np.float32)
            for jf, val in ((qb - 1, 1e9), (qb, 2e9), (0, 3e9)):
                if jf >= 0:
                    a_[jf] = 0.0
                    b_[jf] = val
            ta[pp, qt] = a_
            tb[pp, qt] = b_
    c["nsa_ta"] = ta.reshape(128, NT * 64)
    c["nsa_tb"] = tb.reshape(128, NT * 64)
    n = np.arange(256)
    cs = n[:, None] * 16
    bs = blk[None, :] * 64
    ov = ((cs < bs + 64) & (cs + 32 > bs)).astype(np.float32)
    ov[255, :] = 0.0
    c["nsa_ov"] = ov.astype(ml_dtypes.bfloat16)
    c["nsa_id64"] = ((p[:, None] % 64) == np.arange(64)[None, :]).astype(np.float32).astype(ml_dtypes.bfloat16)
    return c


INPUT_SHAPES = {
    "x": [S, D], "norm_g": [DEPTH, 6, D], "ffn_w_in": [DEPTH, 2, D, 2 * DFF], "ffn_w_out": [DEPTH, 2, DFF, D],
    "diff_w_in": [2, D, 3072], "diff_w_out": [2, D, D], "diff_lambda": [2, 4, 64], "diff_subln": [2, 128],
    "moba_w_in": [1, D, 3072], "moba_w_out": [1, D, D],
    "nsa_w_in": [1, D, 2608], "nsa_w_out": [1, D, D], "nsa_cmp_pe": [1, 2, 32, 64],
    "nsa_cmp_w1": [1, 2, 2048, 256], "nsa_cmp_w2": [1, 2, 256, 64],
}


def build_program(phases, used=None):
    nc = bass.Bass("TRN2", target_bir_lowering=False)
    io = {}
    for name, shape in INPUT_SHAPES.items():
        if used is not None and name not in used:
            continue
        io[name] = nc.dram_tensor(name, list(shape), F32, kind="ExternalInput").ap()
    for name, arr in host_consts().items():
        dt = BF16 if arr.dtype == ml_dtypes.bfloat16 else F32
        io[name] = nc.dram_tensor(name, list(arr.shape), dt, kind="ExternalInput").ap()
    io["out"] = nc.dram_tensor("out", [S, D], F32, kind="ExternalOutput").ap()
    io["H"] = nc.dram_tensor("H", [S, D], F32, kind="Internal").ap()
    io["FM"] = nc.dram_tensor("FM", [2560, S], BF16, kind="Internal").ap()
    io["TM"] = nc.dram_tensor("TM", [S, 1024], BF16, kind="Internal").ap()
    io["OT"] = nc.dram_tensor("OT", [D, S], BF16, kind="Internal").ap()
    fw = FW(nc)
    pg = Prog(nc, fw, io)
    phases(pg, io)
    fw.close()
    return nc, fw


def lambda_init_of(i):
    return 0.8 - 0.6 * math.exp(-0.3 * i)


def mixer_diff(pg, io, i, src, dst):
    j = i // 3
    segs = [("rope", k) for k in range(8)] + [("tm", k * 256) for k in range(4)]
    pg.proj(src, io["diff_w_in"][j], 3072, io["norm_g"][i, 2], segs, io["FM"][0:2048, :], io["TM"],
            (io["rope_cc"], io["rope_ss"]))
    pg.diff_attn(io["FM"][0:2048, :], io["TM"], io["OT"], io["diff_lambda"][j], io["diff_subln"][j],
                 lambda_init_of(i), io["tri"])
    pg.outproj(src, dst, io["OT"], io["diff_w_out"][j], io["norm_g"][i, 3])


def mixer_moba(pg, io, i, src, dst):
    j = i // 3
    segs = [("rope", k) for k in range(8)] + [("tm", k * 256) for k in range(4)]
    pg.proj(src, io["moba_w_in"][j], 3072, io["norm_g"][i, 2], segs, io["FM"][0:2048, :], io["TM"],
            (io["rope_cc"], io["rope_ss"]))
    pg.moba_attn(io["FM"][0:2048, :], io["TM"], io["OT"], io["tri"], io["moba_vb"], io["moba_e16"])
    pg.outproj(src, dst, io["OT"], io["moba_w_out"][j], io["norm_g"][i, 3])


def mixer_nsa(pg, io, i, src, dst):
    j = i // 3
    segs = [("rope", 0), ("rope", 1), ("rope", 2), ("rope", 3), ("rope", 4), ("fm", 5), ("rope", 6), ("tm", 0),
            ("rope", 7), ("tm", 256), ("gate", 8)]
    pg.proj(src, io["nsa_w_in"][j], 2608, io["norm_g"][i, 2], segs, io["FM"][0:2304, :], io["TM"],
            (io["rope_cc"], io["rope_ss"]))
    pg.nsa_attn(io["FM"], io["TM"], io["OT"], io["nsa_cmp_pe"][j], io["nsa_cmp_w1"][j], io["nsa_cmp_w2"][j], io)
    pg.outproj(src, dst, io["OT"], io["nsa_w_out"][j], io["norm_g"][i, 3])


MIXERS = {0: mixer_diff, 1: mixer_moba, 2: mixer_nsa}


def full_phases(pg, io):
    H = io["H"]
    for i in range(DEPTH):
        src = io["x"] if i == 0 else H
        pg.ffn(src, H, io["ffn_w_in"][i, 0], io["ffn_w_out"][i, 0], io["norm_g"][i, 0], io["norm_g"][i, 1])
        MIXERS[i % 3](pg, io, i, H, H)
        dst = io["out"] if i == DEPTH - 1 else H
        pg.ffn(H, dst, io["ffn_w_in"][i, 1], io["ffn_w_out"][i, 1], io["norm_g"][i, 4], io["norm_g"][i, 5])


_CACHE = {}


def kernel(**inputs):
    if "nc" not in _CACHE:
        _CACHE["nc"] = build_program(full_phases)[0]
        _CACHE["consts"] = host_consts()
    nc = _CACHE["nc"]
    consts = _CACHE["consts"]
    arrs = {k: np.ascontiguousarray(np.asarray(v), dtype=np.float32) for k, v in inputs.items()}
    n = arrs["x"].shape[0]
    in_maps = []
    for b in range(n):
        m = {k: v for k, v in arrs.items() if k != "x"}
        m["x"] = arrs["x"][b]
        m.update(consts)
        in_maps.append(m)
    res = run_bass_kernel_spmd(nc, in_maps, core_ids=list(range(n)))
    return np.stack([np.asarray(r["out"], dtype=np.float32) for r in res.results], axis=0)
```

```python
from contextlib import ExitStack
import math
import numpy as np
import ml_dtypes
import concourse.bass as bass
import concourse.mybir as mybir
from concourse.bass_utils import run_bass_kernel_spmd

F32 = mybir.dt.float32
BF16 = mybir.dt.bfloat16
AF = mybir.ActivationFunctionType
ALU = mybir.AluOpType
AX = mybir.AxisListType

S = 4096
D = 1024
DFF = 2816
DEPTH = 4
NT = S // 128
HD = 64
NEG = -30000.0
EPS = 1e-6


class Buf:
    __slots__ = ("w", "r")

    def __init__(self):
        self.w = None
        self.r = []


class SemCtr:
    __slots__ = ("sem", "val", "idx")

    def __init__(self, sem, idx):
        self.sem = sem
        self.val = 0
        self.idx = idx


class Eng:
    def __init__(self, fw, name, eng, is_compute=True, self_sync=True):
        self.fw = fw
        self.name = name
        self.eng = eng
        self.seen = {}
        self.self_sync = self_sync
        self.ctr = fw.new_sem("s_" + name) if is_compute else None
        self.dma_sems = []
        self.dma_rr = 0

    def _wait(self, tok, raw=True):
        s, v = tok
        if s is self.ctr and not (self.self_sync and raw):
            return
        if self.seen.get(s.idx, 0) >= v:
            return
        self.eng.wait_ge(s.sem, v)
        self.seen[s.idx] = v
        self.fw.n_wait += 1

    def deps(self, reads, writes):
        for b in reads:
            if b.w is not None:
                self._wait(b.w, True)
        for b in writes:
            if b.w is not None:
                self._wait(b.w, False)
            for t in b.r:
                self._wait(t, False)

    def done(self, tok, reads, writes):
        for b in writes:
            b.w = tok
            b.r = []
        for b in reads:
            if b in writes:
                continue
            b.r = [t for t in b.r if t[0] is not tok[0]] + [tok]

    def op(self, fn, reads=(), writes=()):
        self.deps(reads, writes)
        inst = fn()
        self.ctr.val += 1
        inst.then_inc(self.ctr.sem, 1)
        tok = (self.ctr, self.ctr.val)
        self.done(tok, reads, writes)
        self.fw.n_inst += 1
        return tok

    def dma(self, out, in_, reads=(), writes=(), **kw):
        if not self.dma_sems:
            self.dma_sems = [self.fw.new_sem(f"d_{self.name}{i}") for i in range(self.fw.n_dma_sems)]
        s = self.dma_sems[self.dma_rr % len(self.dma_sems)]
        self.dma_rr += 1
        if s.val > 0:
            self._wait((s, s.val))
        self.deps(reads, writes)
        inst = self.eng.dma_start(out=out, in_=in_, **kw)
        s.val += 16
        inst.then_inc(s.sem, 16)
        tok = (s, s.val)
        self.done(tok, reads, writes)
        self.fw.n_inst += 1
        return tok


class Tile:
    def __init__(self, t):
        self.t = t
        self.b = Buf()

    def __getitem__(self, idx):
        return self.t[idx]


class FW:
    def __init__(self, nc, n_dma_sems=8):
        self.nc = nc
        self.stack = [ExitStack()]
        self.n_dma_sems = n_dma_sems
        self.n_sem = 0
        self.n_inst = 0
        self.n_wait = 0
        self.sems = []
        self.pe = Eng(self, "pe", nc.tensor, self_sync=False)
        self.act = Eng(self, "act", nc.scalar)
        self.dve = Eng(self, "dve", nc.vector)
        self.pool = Eng(self, "pool", nc.gpsimd)
        self.sp = Eng(self, "sp", nc.sync, is_compute=False)
        self.engs = [self.pe, self.act, self.dve, self.pool, self.sp]
        self._uid = 0

    def new_sem(self, name):
        h = self.stack[0].enter_context(self.nc.semaphore(name))
        self.n_sem += 1
        sc = SemCtr(h, self.n_sem)
        self.sems.append(sc)
        return sc

    def push(self):
        self.stack.append(ExitStack())

    def pop(self):
        self.barrier()
        self.stack.pop().close()
        for e in (self.pe, self.act):
            e.ctr = self.new_sem(f"s_{e.name}_{self.n_sem}")

    def barrier(self):
        for e in self.engs:
            for s in self.sems:
                if s.val > 0:
                    e._wait((s, s.val), True)

    def sb(self, shape, dtype):
        self._uid += 1
        return Tile(self.stack[-1].enter_context(self.nc.sbuf_tensor(f"sb{self._uid}", list(shape), dtype)))

    def ps(self, shape, dtype):
        self._uid += 1
        return Tile(self.stack[-1].enter_context(self.nc.psum_tensor(f"ps{self._uid}", list(shape), dtype)))

    def close(self):
        self.barrier()
        while self.stack:
            self.stack.pop().close()


class Prog:
    def __init__(self, nc, fw, io):
        self.nc = nc
        self.fw = fw
        self.io = io
        nc_ = nc
        fw_ = fw
        self.ident = fw.sb([128, 128], BF16)
        self.identf = fw.sb([128, 128], F32)
        self.mhalf = fw.sb([128, 1], F32)
        self.ones_bf = fw.sb([128, 128], BF16)
        self.junk = fw.sb([128, D], BF16)
        fw.pool.op(lambda: nc_.gpsimd.memset(self.identf[:], 1.0), writes=[self.identf.b])
        fw.pool.op(lambda: nc_.gpsimd.affine_select(out=self.identf[:], in_=self.identf[:], pattern=[[-1, 128]],
                                                     compare_op=ALU.is_equal, fill=0.0, base=0, channel_multiplier=1),
                   reads=[self.identf.b], writes=[self.identf.b])
        fw.dve.op(lambda: nc_.vector.tensor_copy(out=self.ident[:], in_=self.identf[:]),
                  reads=[self.identf.b], writes=[self.ident.b])
        fw.dve.op(lambda: nc_.vector.memset(self.mhalf[:], -0.5), writes=[self.mhalf.b])
        fw.dve.op(lambda: nc_.vector.memset(self.ones_bf[:], 1.0), writes=[self.ones_bf.b])

    def rstd(self, ss, out, mul, eng_pool=True):
        raise NotImplementedError

    def load_gT(self, dst, vec_ap):
        self.fw.sp.dma(dst[:], vec_ap.rearrange("(k p) -> p k", p=128), writes=[dst.b],
                       allow_slow_non_contiguous=True)

    def load_bc(self, dst, row_ap):
        self.fw.sp.dma(dst[:], row_ap.partition_broadcast(128), writes=[dst.b])

    def norm_to_xnT(self, tt, src, g_T, htile, ss, var, rs, xn, ptr, xnT, dmaq=None):
        nc, fw = self.nc, self.fw
        fw.sp.dma(htile[:], src[tt * 128:(tt + 1) * 128, :], writes=[htile.b])
        fw.act.op(lambda: nc.scalar.activation(out=self.junk[:], in_=htile[:], func=AF.Square, accum_out=ss[:]),
                  reads=[htile.b], writes=[self.junk.b, ss.b])
        fw.dve.op(lambda: nc.vector.tensor_scalar(out=var[:], in0=ss[:], scalar1=1.0 / D, scalar2=EPS,
                                                  op0=ALU.mult, op1=ALU.add), reads=[ss.b], writes=[var.b])
        fw.pool.op(lambda: nc.gpsimd.tensor_tensor(out=rs[:], in0=var[:], in1=self.mhalf[:], op=ALU.pow),
                   reads=[var.b, self.mhalf.b], writes=[rs.b])
        fw.pool.op(lambda: nc.gpsimd.tensor_scalar(out=xn[:], in0=htile[:], scalar1=rs[:], scalar2=None,
                                                   op0=ALU.mult), reads=[htile.b, rs.b], writes=[xn.b])
        for k in range(8):
            fw.pe.op(lambda k=k: nc.tensor.transpose(out=ptr[:, k, :], in_=xn[:, k * 128:(k + 1) * 128],
                                                     identity=self.ident[:]),
                     reads=[xn.b, self.ident.b], writes=[ptr.b])
        fw.dve.op(lambda: nc.vector.tensor_tensor(out=xnT[:], in0=ptr[:, 0:8, :],
                                                  in1=g_T[:, :, None].to_broadcast([128, 8, 128]), op=ALU.mult),
                  reads=[ptr.b, g_T.b], writes=[xnT.b])

    def ffn(self, src, dst, w_in, w_out, g_pre, g_post):
        nc, fw = self.nc, self.fw
        fw.push()
        win = [fw.sb([128, 2 * DFF], BF16) for _ in range(8)]
        wout = [fw.sb([128, D], BF16) for _ in range(22)]
        for k in range(8):
            fw.pool.dma(win[k][:], w_in[k * 128:(k + 1) * 128, :], writes=[win[k].b])
        for c in range(22):
            fw.pool.dma(wout[c][:], w_out[c * 128:(c + 1) * 128, :], writes=[wout[c].b])
        gT = fw.sb([128, 8], F32)
        self.load_gT(gT, g_pre)
        gbc = fw.sb([128, D], F32)
        self.load_bc(gbc, g_post)
        NH = 3
        htiles = [fw.sb([128, D], F32) for _ in range(NH)]
        xn = fw.sb([128, D], BF16)
        xnT = fw.sb([128, 8, 128], BF16)
        sg = [fw.sb([128, 512], BF16) for _ in range(2)]
        act = fw.sb([128, DFF], BF16)
        actT = fw.sb([128, 22, 128], BF16)
        tt_ = [fw.sb([128, D], F32) for _ in range(2)]
        ss = [fw.sb([128, 1], F32) for _ in range(2)]
        var = [fw.sb([128, 1], F32) for _ in range(2)]
        rs = [fw.sb([128, 1], F32) for _ in range(2)]
        ss2 = [fw.sb([128, 1], F32) for _ in range(2)]
        var2 = [fw.sb([128, 1], F32) for _ in range(2)]
        rs2 = [fw.sb([128, 1], F32) for _ in range(2)]
        junk = self.junk
        pg = [fw.ps([128, 512], F32) for _ in range(2)]
        pu = [fw.ps([128, 512], F32) for _ in range(2)]
        py = fw.ps([128, D], F32)
        ptr = [fw.ps([128, 8, 128], BF16) for _ in range(2)]
        groups = [(c0, 512) for c0 in range(0, 2560, 512)] + [(2560, 256)]
        state = {"gi": 0, "tr": 0}

        def stage_norm(i):
            p = ptr[state["tr"] % 2]
            state["tr"] += 1
            self.norm_to_xnT(i, src, gT, htiles[i % NH], ss[i % 2], var[i % 2], rs[i % 2], xn, p, xnT)

        def stage_m1(i):
            for (c0, w) in groups:
                gi = state["gi"]
                state["gi"] += 1
                a, b_ = pg[gi % 2], pu[gi % 2]
                for k in range(8):
                    fw.pe.op(lambda k=k: nc.tensor.matmul(a[:, 0:w], lhsT=xnT[:, k, :], rhs=win[k][:, c0:c0 + w],
                                                          start=(k == 0), stop=(k == 7)),
                             reads=[xnT.b, win[k].b], writes=[a.b])
                for k in range(8):
                    fw.pe.op(lambda k=k: nc.tensor.matmul(b_[:, 0:w], lhsT=xnT[:, k, :],
                                                          rhs=win[k][:, DFF + c0:DFF + c0 + w],
                                                          start=(k == 0), stop=(k == 7)),
                             reads=[xnT.b, win[k].b], writes=[b_.b])
                s_ = sg[gi % 2]
                fw.act.op(lambda: nc.scalar.activation(out=s_[:, 0:w], in_=a[:, 0:w], func=AF.Silu),
                          reads=[a.b], writes=[s_.b])
                fw.dve.op(lambda: nc.vector.tensor_tensor(out=act[:, c0:c0 + w], in0=s_[:, 0:w], in1=b_[:, 0:w],
                                                          op=ALU.mult), reads=[s_.b, b_.b], writes=[act.b])

        def stage_t22(i):
            for r0 in (0, 8, 16):
                n = min(8, 22 - r0)
                p = ptr[state["tr"] % 2]
                state["tr"] += 1
                for c in range(n):
                    fw.pe.op(lambda c=c: nc.tensor.transpose(out=p[:, c, :], in_=act[:, (r0 + c) * 128:(r0 + c + 1) * 128],
                                                             identity=self.ident[:]),
                             reads=[act.b, self.ident.b], writes=[p.b])
                if r0 == 8:
                    fw.act.op(lambda: nc.scalar.copy(out=actT[:, r0:r0 + n, :], in_=p[:, 0:n, :]),
                              reads=[p.b], writes=[actT.b])
                else:
                    fw.dve.op(lambda: nc.vector.tensor_copy(out=actT[:, r0:r0 + n, :], in_=p[:, 0:n, :]),
                              reads=[p.b], writes=[actT.b])

        def stage_m2(i):
            for hh in range(2):
                for c in range(22):
                    fw.pe.op(lambda c=c: nc.tensor.matmul(py[:, hh * 512:(hh + 1) * 512], lhsT=actT[:, c, :],
                                                          rhs=wout[c][:, hh * 512:(hh + 1) * 512],
                                                          start=(c == 0), stop=(c == 21)),
                             reads=[actT.b, wout[c].b], writes=[py.b])
            j = i % 2
            ht = htiles[i % NH]
            fw.act.op(lambda: nc.scalar.activation(out=junk[:], in_=py[:], func=AF.Square, accum_out=ss2[j][:]),
                      reads=[py.b], writes=[junk.b, ss2[j].b])
            fw.dve.op(lambda: nc.vector.tensor_scalar(out=var2[j][:], in0=ss2[j][:], scalar1=1.0 / D, scalar2=EPS,
                                                      op0=ALU.mult, op1=ALU.add), reads=[ss2[j].b], writes=[var2[j].b])
            fw.pool.op(lambda: nc.gpsimd.tensor_tensor(out=rs2[j][:], in0=var2[j][:], in1=self.mhalf[:], op=ALU.pow),
                       reads=[var2[j].b, self.mhalf.b], writes=[rs2[j].b])
            t = tt_[j]
            fw.dve.op(lambda: nc.vector.tensor_tensor(out=t[:], in0=py[:], in1=gbc[:], op=ALU.mult),
                      reads=[py.b, gbc.b], writes=[t.b])
            fw.pool.op(lambda: nc.gpsimd.tensor_scalar(out=t[:], in0=t[:], scalar1=rs2[j][:], scalar2=0.5,
                                                       op0=ALU.mult, op1=ALU.mult),
                       reads=[t.b, rs2[j].b], writes=[t.b])
            fw.pool.op(lambda: nc.gpsimd.tensor_tensor(out=t[:], in0=t[:], in1=ht[:], op=ALU.add),
                       reads=[t.b, ht.b], writes=[t.b])
            fw.sp.dma(dst[i * 128:(i + 1) * 128, :], t[:], reads=[t.b])

        stage_norm(0)
        for i in range(NT):
            stage_m1(i)
            if i + 1 < NT:
                stage_norm(i + 1)
            if i >= 1:
                stage_m2(i - 1)
            stage_t22(i)
        stage_m2(NT - 1)
        fw.pop()


    def proj(self, src, w_in, W, g_pre, segs, fm_dst, tm_dst, cs_tab):
        nc, fw = self.nc, self.fw
        fw.push()
        wt = [fw.sb([128, W], BF16) for _ in range(8)]
        for k in range(8):
            fw.pool.dma(wt[k][:], w_in[k * 128:(k + 1) * 128, :], writes=[wt[k].b])
        gT = fw.sb([128, 8], F32)
        self.load_gT(gT, g_pre)
        cc = fw.sb([128, NT, 16], F32)
        sn = fw.sb([128, NT, 16], F32)
        fw.sp.dma(cc[:], cs_tab[0].rearrange("(t p) c -> p t c", p=128), writes=[cc.b])
        fw.sp.dma(sn[:], cs_tab[1].rearrange("(t p) c -> p t c", p=128), writes=[sn.b])
        nseg = len(segs)
        nfm = sum(1 for kd, _ in segs if kd in ("rope", "fm", "gate")) * 2
        ntm = sum(1 for kd, _ in segs if kd == "tm") * 256
        htiles = [fw.sb([128, D], F32) for _ in range(2)]
        xn = fw.sb([128, D], BF16)
        xnT = [fw.sb([128, 8, 128], BF16) for _ in range(2)]
        ss = [fw.sb([128, 1], F32) for _ in range(2)]
        var = [fw.sb([128, 1], F32) for _ in range(2)]
        rs = [fw.sb([128, 1], F32) for _ in range(2)]
        tm = [fw.sb([128, nseg * 256], BF16) for _ in range(2)]
        ra = [fw.sb([128, 4, 16], F32) for _ in range(2)]
        rb = [fw.sb([128, 4, 16], F32) for _ in range(2)]
        stT = [fw.sb([128, nfm, 512], BF16) for _ in range(2)]
        ptr = fw.ps([128, 8, 128], BF16)
        ptr2 = [fw.ps([128, 8, 128], BF16) for _ in range(2)]
        pp = [fw.ps([128, 512], F32) for _ in range(3)]
        ngrp = (nseg + 1) // 2
        cnt = {"g": 0, "t": 0, "r": 0}
        fm_view = fm_dst.rearrange("(c p) t -> p c t", p=128)
        for i in range(NT):
            xT = xnT[i % 2]
            self.norm_to_xnT(i, src, gT, htiles[i % 2], ss[i % 2], var[i % 2], rs[i % 2], xn, ptr, xT)
            tmi = tm[i % 2]
            for g in range(ngrp):
                c0 = g * 512
                w = min(512, W - c0)
                p_ = pp[cnt["g"] % 3]
                cnt["g"] += 1
                for k in range(8):
                    fw.pe.op(lambda k=k: nc.tensor.matmul(p_[:, 0:w], lhsT=xT[:, k, :], rhs=wt[k][:, c0:c0 + w],
                                                          start=(k == 0), stop=(k == 7)),
                             reads=[xT.b, wt[k].b], writes=[p_.b])
                for sidx in (2 * g, 2 * g + 1):
                    if sidx >= nseg:
                        continue
                    kind = segs[sidx][0]
                    lo = (sidx % 2) * 256
                    wseg = min(256, w - lo)
                    dstv = tmi[:, sidx * 256: sidx * 256 + wseg]
                    if kind == "rope":
                        x3 = p_[:, lo:lo + 256].rearrange("p (h d) -> p h d", d=64)
                        o3 = tmi[:, sidx * 256:(sidx + 1) * 256].rearrange("p (h d) -> p h d", d=64)
                        a_, b_ = ra[cnt["r"] % 2], rb[cnt["r"] % 2]
                        cnt["r"] += 1
                        ccb = cc[:, i, :][:, None, :].to_broadcast([128, 4, 16])
                        fw.dve.op(lambda: nc.vector.tensor_tensor(out=a_[:], in0=x3[:, :, 0:16], in1=ccb, op=ALU.mult),
                                  reads=[p_.b, cc.b], writes=[a_.b])
                        fw.dve.op(lambda: nc.vector.tensor_tensor(out=b_[:, :, 0:8], in0=x3[:, :, 8:16],
                                                                  in1=sn[:, i, 0:8][:, None, :].to_broadcast([128, 4, 8]),
                                                                  op=ALU.mult), reads=[p_.b, sn.b], writes=[b_.b])
                        fw.dve.op(lambda: nc.vector.tensor_tensor(out=b_[:, :, 8:16], in0=x3[:, :, 0:8],
                                                                  in1=sn[:, i, 8:16][:, None, :].to_broadcast([128, 4, 8]),
                                                                  op=ALU.mult), reads=[p_.b, sn.b], writes=[b_.b])
                        fw.pool.op(lambda: nc.gpsimd.tensor_tensor(out=o3[:, :, 0:16], in0=a_[:], in1=b_[:], op=ALU.add),
                                   reads=[a_.b, b_.b], writes=[tmi.b])
                        fw.act.op(lambda: nc.scalar.copy(out=o3[:, :, 16:64], in_=x3[:, :, 16:64]),
                                  reads=[p_.b], writes=[tmi.b])
                    elif kind == "gate":
                        fw.act.op(lambda: nc.scalar.activation(out=dstv, in_=p_[:, lo:lo + wseg], func=AF.Sigmoid),
                                  reads=[p_.b], writes=[tmi.b])
                    else:
                        fw.act.op(lambda: nc.scalar.copy(out=dstv, in_=p_[:, lo:lo + wseg]),
                                  reads=[p_.b], writes=[tmi.b])
            for sidx, (kind, di) in enumerate(segs):
                if kind == "tm":
                    fw.sp.dma(tm_dst[i * 128:(i + 1) * 128, di:di + 256], tmi[:, sidx * 256:(sidx + 1) * 256],
                              reads=[tmi.b])
            st = stT[(i // 4) % 2]
            tcol = (i % 4) * 128
            pend = []
            for sidx, (kind, di) in enumerate(segs):
                if kind in ("rope", "fm", "gate"):
                    wseg = min(256, W - sidx * 256)
                    for hh in range(2):
                        wc = min(128, wseg - hh * 128)
                        if wc <= 0:
                            continue
                        pend.append((sidx * 256 + hh * 128, wc, di * 2 + hh))
            for j0 in range(0, len(pend), 8):
                grp = pend[j0:j0 + 8]
                p2 = ptr2[cnt["t"] % 2]
                cnt["t"] += 1
                for jj, (col, wc, ch) in enumerate(grp):
                    fw.pe.op(lambda jj=jj, col=col, wc=wc: nc.tensor.transpose(out=p2[0:wc, jj, :], in_=tmi[:, col:col + wc],
                                                                             identity=self.ident[:]),
                             reads=[tmi.b, self.ident.b], writes=[p2.b])
                ch0 = grp[0][2]
                contiguous = all(grp[jj][2] == ch0 + jj and grp[jj][1] == 128 for jj in range(len(grp)))
                if contiguous:
                    eng = fw.dve if (cnt["t"] % 2) else fw.act
                    if eng is fw.dve:
                        fw.dve.op(lambda: nc.vector.tensor_copy(out=st[:, ch0:ch0 + len(grp), tcol:tcol + 128],
                                                                in_=p2[:, 0:len(grp), :]), reads=[p2.b], writes=[st.b])
                    else:
                        fw.act.op(lambda: nc.scalar.copy(out=st[:, ch0:ch0 + len(grp), tcol:tcol + 128],
                                                         in_=p2[:, 0:len(grp), :]), reads=[p2.b], writes=[st.b])
                else:
                    for jj, (col, wc, ch) in enumerate(grp):
                        fw.dve.op(lambda jj=jj, wc=wc, ch=ch: nc.vector.tensor_copy(out=st[0:wc, ch, tcol:tcol + 128],
                                                                                  in_=p2[0:wc, jj, :]),
                                  reads=[p2.b], writes=[st.b])
            if i % 4 == 3:
                t0 = (i // 4) * 512
                fw.sp.dma(fm_view[:, :, t0:t0 + 512], st[:], reads=[st.b])
        fw.pop()

    def outproj(self, src, dst, oT_dram, w_out, g_post):
        nc, fw = self.nc, self.fw
        fw.push()
        wo = [fw.sb([128, D], BF16) for _ in range(8)]
        for c in range(8):
            fw.pool.dma(wo[c][:], w_out[c * 128:(c + 1) * 128, :], writes=[wo[c].b])
        gbc = fw.sb([128, D], F32)
        self.load_bc(gbc, g_post)
        oT = [fw.sb([128, 8, 512], BF16) for _ in range(2)]
        htiles = [fw.sb([128, D], F32) for _ in range(3)]
        tt_ = [fw.sb([128, D], F32) for _ in range(2)]
        ss2 = [fw.sb([128, 1], F32) for _ in range(2)]
        var2 = [fw.sb([128, 1], F32) for _ in range(2)]
        rs2 = [fw.sb([128, 1], F32) for _ in range(2)]
        py = [fw.ps([128, D], F32) for _ in range(2)]
        ov = oT_dram.rearrange("(c p) t -> p c t", p=128)
        for i in range(NT):
            o_ = oT[(i // 4) % 2]
            if i % 4 == 0:
                fw.sp.dma(o_[:], ov[:, :, i * 128:i * 128 + 512], writes=[o_.b])
            ht = htiles[i % 3]
            fw.sp.dma(ht[:], src[i * 128:(i + 1) * 128, :], writes=[ht.b])
            y = py[i % 2]
            tc = (i % 4) * 128
            for hh in range(2):
                for c in range(8):
                    fw.pe.op(lambda c=c: nc.tensor.matmul(y[:, hh * 512:(hh + 1) * 512], lhsT=o_[:, c, tc:tc + 128],
                                                          rhs=wo[c][:, hh * 512:(hh + 1) * 512],
                                                          start=(c == 0), stop=(c == 7)),
                             reads=[o_.b, wo[c].b], writes=[y.b])
            j = i % 2
            fw.act.op(lambda: nc.scalar.activation(out=self.junk[:], in_=y[:], func=AF.Square, accum_out=ss2[j][:]),
                      reads=[y.b], writes=[self.junk.b, ss2[j].b])
            fw.dve.op(lambda: nc.vector.tensor_scalar(out=var2[j][:], in0=ss2[j][:], scalar1=1.0 / D, scalar2=EPS,
                                                      op0=ALU.mult, op1=ALU.add), reads=[ss2[j].b], writes=[var2[j].b])
            fw.pool.op(lambda: nc.gpsimd.tensor_tensor(out=rs2[j][:], in0=var2[j][:], in1=self.mhalf[:], op=ALU.pow),
                       reads=[var2[j].b, self.mhalf.b], writes=[rs2[j].b])
            t = tt_[j]
            fw.dve.op(lambda: nc.vector.tensor_tensor(out=t[:], in0=y[:], in1=gbc[:], op=ALU.mult),
                      reads=[y.b, gbc.b], writes=[t.b])
            fw.dve.op(lambda: nc.vector.scalar_tensor_tensor(out=t[:], in0=t[:], scalar=rs2[j][:], in1=ht[:],
                                                             op0=ALU.mult, op1=ALU.add),
                      reads=[t.b, rs2[j].b, ht.b], writes=[t.b])
            fw.sp.dma(dst[i * 128:(i + 1) * 128, :], t[:], reads=[t.b])
        fw.pop()

    def att_state(self, n_s=3, n_pt=5, lag=2):
        fw = self.fw
        return {"i": 0, "ps_s": [fw.ps([128, 512], F32) for _ in range(n_s)],
                "pt": [fw.sb([128, 512], BF16) for _ in range(n_pt)], "pend": [], "lag": lag}

    def att_step(self, st, kT, qT, c0, c1, outs, start, rd, bias=None, masks=(), scale=0.125):
        nc, fw = self.nc, self.fw
        i = st["i"]
        st["i"] += 1
        S_ = st["ps_s"][i % len(st["ps_s"])]
        pt = st["pt"][i % len(st["pt"])]
        fw.pe.op(lambda: nc.tensor.matmul(S_[:, c0:c1], lhsT=kT, rhs=qT[:, c0:c1], start=True, stop=(bias is None)),
                 reads=rd, writes=[S_.b])
        if bias is not None:
            bl, br, b0, b1, brd = bias
            fw.pe.op(lambda: nc.tensor.matmul(S_[:, b0:b1], lhsT=bl, rhs=br[:, b0:b1], start=False, stop=True,
                                              skip_group_check=True), reads=brd, writes=[S_.b])
        fw.act.op(lambda: nc.scalar.activation(out=pt[:, c0:c1], in_=S_[:, c0:c1], func=AF.Exp, scale=scale),
                  reads=[S_.b], writes=[pt.b])
        for (m0, mw, mt, mc) in masks:
            fw.pool.op(lambda m0=m0, mw=mw, mt=mt, mc=mc: nc.gpsimd.tensor_tensor(
                out=pt[:, m0:m0 + mw], in0=pt[:, m0:m0 + mw], in1=mt[:, mc:mc + mw], op=ALU.mult),
                reads=[pt.b, mt.b], writes=[pt.b])

        def stage_c():
            for (po, lhsT, lrd) in outs:
                fw.pe.op(lambda po=po, lhsT=lhsT: nc.tensor.matmul(po[:, c0:c1], lhsT=lhsT, rhs=pt[:, c0:c1],
                                                                   start=start, stop=False, skip_group_check=True),
                         reads=[pt.b] + lrd, writes=[po.b])
        st["pend"].append(stage_c)
        while len(st["pend"]) > st["lag"]:
            st["pend"].pop(0)()

    def att_flush(self, st):
        while st["pend"]:
            st["pend"].pop(0)()

    def diff_attn(self, qkT, v_tm, oT_dram, lam_ap, subln_ap, lambda_init, tri_ap):
        nc, fw = self.nc, self.fw
        fw.push()
        tri = fw.sb([128, 128], BF16)
        fw.sp.dma(tri[:], tri_ap, writes=[tri.b])
        lam = fw.sb([128, 256], F32)
        fw.sp.dma(lam[:], lam_ap.rearrange("a d -> (a d)").partition_broadcast(128), writes=[lam.b])
        lj = fw.sb([128, 64], F32)
        s01 = fw.sb([128, 1], F32)
        s23 = fw.sb([128, 1], F32)
        nlam = fw.sb([128, 1], F32)
        fw.dve.op(lambda: nc.vector.scalar_tensor_tensor(out=lj[:], in0=lam[:, 0:64], scalar=1.0, in1=lam[:, 64:128],
                                                         op0=ALU.mult, op1=ALU.mult, accum_out=s01[:]),
                  reads=[lam.b], writes=[lj.b, s01.b])
        fw.dve.op(lambda: nc.vector.scalar_tensor_tensor(out=lj[:], in0=lam[:, 128:192], scalar=1.0, in1=lam[:, 192:256],
                                                         op0=ALU.mult, op1=ALU.mult, accum_out=s23[:]),
                  reads=[lam.b], writes=[lj.b, s23.b])
        fw.act.op(lambda: nc.scalar.activation(out=s01[:], in_=s01[:], func=AF.Exp), reads=[s01.b], writes=[s01.b])
        fw.act.op(lambda: nc.scalar.activation(out=s23[:], in_=s23[:], func=AF.Exp), reads=[s23.b], writes=[s23.b])
        fw.dve.op(lambda: nc.vector.tensor_tensor(out=nlam[:], in0=s23[:], in1=s01[:], op=ALU.subtract),
                  reads=[s01.b, s23.b], writes=[nlam.b])
        fw.dve.op(lambda: nc.vector.tensor_scalar(out=nlam[:], in0=nlam[:], scalar1=-float(lambda_init), scalar2=None,
                                                  op0=ALU.add), reads=[nlam.b], writes=[nlam.b])
        gs = fw.sb([128, 1], F32)
        fw.sp.dma(gs[:], subln_ap.rearrange("(p o) -> p o", o=1), writes=[gs.b])
        fw.dve.op(lambda: nc.vector.tensor_scalar(out=gs[:], in0=gs[:], scalar1=float(1.0 - lambda_init), scalar2=None,
                                                  op0=ALU.mult), reads=[gs.b], writes=[gs.b])
        qT = [fw.sb([128, S], BF16) for _ in range(2)]
        kT = [fw.sb([128, S], BF16) for _ in range(2)]
        vv = fw.sb([128, NT, D], BF16)
        v_view = v_tm.rearrange("(t p) c -> p t c", p=128)
        for t0 in range(0, NT, 8):
            fw.sp.dma(vv[:, t0:t0 + 8, :], v_view[:, t0:t0 + 8, :], writes=[vv.b])
        st = self.att_state()
        pO = [fw.ps([128, 512], F32) for _ in range(2)]
        pS = [fw.ps([128, 512], F32) for _ in range(2)]
        pQ = pO[0] if False else fw.ps([128, 512], F32)
        r0 = fw.sb([128, 512], F32)
        r1 = fw.sb([128, 512], F32)
        a0 = fw.sb([128, 512], F32)
        a1 = fw.sb([128, 512], F32)
        sq = fw.sb([128, 512], BF16)
        rst = fw.sb([128, 512], F32)
        ob = [fw.sb([128, 512], BF16) for _ in range(2)]
        import os
        nhead = int(os.environ.get('NHEAD', 8))
        for h in range(int(os.environ.get('HSTART', 0)), nhead):
            q_, k_ = qT[h % 2], kT[h % 2]
            fw.sp.dma(q_[:], qkT[h * 128:(h + 1) * 128, :], writes=[q_.b])
            fw.sp.dma(k_[:], qkT[1024 + h * 128:1024 + (h + 1) * 128, :], writes=[k_.b])
            for qg in range(8):
                for c in range(2):
                    nk = 4 * qg + 4
                    for kt in range(nk):
                        v = kt - 4 * qg
                        c0 = 128 * v if v > 0 else 0
                        self.att_step(st, k_[c * 64:(c + 1) * 64, kt * 128:(kt + 1) * 128],
                                      q_[c * 64:(c + 1) * 64, qg * 512:(qg + 1) * 512], c0, 512,
                                      [(pO[c], vv[:, kt, h * 128:(h + 1) * 128], [vv.b]),
                                       (pS[c], self.ones_bf[:], [self.ones_bf.b])],
                                      start=(kt == 0), rd=[q_.b, k_.b],
                                      masks=([(c0, 128, tri, 0)] if v >= 0 else ()))
                self.att_flush(st)
                fw.dve.op(lambda: nc.vector.tensor_copy(out=r0[:], in_=pS[0][:]), reads=[pS[0].b], writes=[r0.b])
                fw.act.op(lambda: nc.scalar.copy(out=a0[:], in_=pO[0][:]), reads=[pO[0].b], writes=[a0.b])
                fw.dve.op(lambda: nc.vector.tensor_copy(out=r1[:], in_=pS[1][:]), reads=[pS[1].b], writes=[r1.b])
                fw.act.op(lambda: nc.scalar.copy(out=a1[:], in_=pO[1][:]), reads=[pO[1].b], writes=[a1.b])
                fw.dve.op(lambda: nc.vector.reciprocal(out=r0[:], in_=r0[:]), reads=[r0.b], writes=[r0.b])
                fw.dve.op(lambda: nc.vector.reciprocal(out=r1[:], in_=r1[:]), reads=[r1.b], writes=[r1.b])
                fw.dve.op(lambda: nc.vector.tensor_tensor(out=a0[:], in0=a0[:], in1=r0[:], op=ALU.mult),
                          reads=[a0.b, r0.b], writes=[a0.b])
                fw.dve.op(lambda: nc.vector.tensor_tensor(out=a1[:], in0=a1[:], in1=r1[:], op=ALU.mult),
                          reads=[a1.b, r1.b], writes=[a1.b])
                fw.dve.op(lambda: nc.vector.scalar_tensor_tensor(out=a0[:], in0=a1[:], scalar=nlam[:], in1=a0[:],
                                                                 op0=ALU.mult, op1=ALU.add),
                          reads=[a0.b, a1.b, nlam.b], writes=[a0.b])
                fw.pool.op(lambda: nc.gpsimd.tensor_tensor(out=sq[:], in0=a0[:], in1=a0[:], op=ALU.mult),
                           reads=[a0.b], writes=[sq.b])
                fw.pe.op(lambda: nc.tensor.matmul(pQ[:], lhsT=self.ones_bf[:], rhs=sq[:], start=True, stop=True),
                         reads=[sq.b, self.ones_bf.b], writes=[pQ.b])
                fw.dve.op(lambda: nc.vector.tensor_scalar(out=rst[:], in0=pQ[:], scalar1=1.0 / 128, scalar2=1e-5,
                                                          op0=ALU.mult, op1=ALU.add), reads=[pQ.b], writes=[rst.b])
                fw.pool.op(lambda: nc.gpsimd.tensor_tensor(out=rst[:], in0=rst[:],
                                                           in1=self.mhalf[:, 0:1].to_broadcast([128, 512]), op=ALU.pow),
                           reads=[rst.b, self.mhalf.b], writes=[rst.b])
                o_ = ob[(h * 8 + qg) % 2]
                fw.dve.op(lambda: nc.vector.scalar_tensor_tensor(out=o_[:], in0=a0[:], scalar=gs[:], in1=rst[:],
                                                                 op0=ALU.mult, op1=ALU.mult),
                          reads=[a0.b, gs.b, rst.b], writes=[o_.b])
                fw.sp.dma(oT_dram[h * 128:(h + 1) * 128, qg * 512:(qg + 1) * 512], o_[:], reads=[o_.b])
            fw.barrier()
        fw.pop()


    def moba_attn(self, qkT, v_tm, oT_dram, tri_ap, vb_ap, e16_ap):
        nc, fw = self.nc, self.fw
        fw.push()
        tri = fw.sb([128, 128], BF16)
        fw.sp.dma(tri[:], tri_ap, writes=[tri.b])
        vb = fw.sb([128, NT, 16], F32)
        fw.sp.dma(vb[:], vb_ap.rearrange("p (t b) -> p t b", b=16), writes=[vb.b])
        e16 = fw.sb([16, 16, 128], BF16)
        fw.sp.dma(e16[:], e16_ap, writes=[e16.b])
        vv = fw.sb([128, NT, D], BF16)
        v_view = v_tm.rearrange("(t p) c -> p t c", p=128)
        for t0 in range(0, NT, 8):
            fw.sp.dma(vv[:, t0:t0 + 8, :], v_view[:, t0:t0 + 8, :], writes=[vv.b])
        qT = [fw.sb([128, S], BF16) for _ in range(2)]
        kT = [fw.sb([128, S], BF16) for _ in range(2)]
        st = self.att_state()
        pO = fw.ps([128, 512], F32)
        pS = fw.ps([128, 512], F32)
        pG = fw.ps([128, NT, 16], F32)
        pT = fw.ps([128, 4, 128], BF16)
        kmf = fw.sb([128, 16], F32)
        kmb = fw.sb([128, 16], BF16)
        gsb = fw.sb([128, NT, 16], F32)
        m8 = fw.sb([128, NT, 8], F32)
        selb = fw.sb([128, NT, 16], F32)
        biasb = [fw.sb([128, NT, 16], BF16) for _ in range(2)]
        sbT = [fw.sb([16, 512], BF16) for _ in range(2)]
        rr = fw.sb([128, 512], F32)
        xo = fw.sb([128, 512], F32)
        ob = [fw.sb([128, 512], BF16) for _ in range(2)]
        cnt = 0
        for j in range(8):
            q_, k_ = qT[j % 2], kT[j % 2]
            fw.sp.dma(q_[:], qkT[j * 128:(j + 1) * 128, :], writes=[q_.b])
            fw.sp.dma(k_[:], qkT[1024 + j * 128:1024 + (j + 1) * 128, :], writes=[k_.b])
            for hh in range(2):
                b0_, b1_ = hh * 64, hh * 64 + 64
                bb = biasb[hh]
                fw.dve.op(lambda: nc.vector.tensor_reduce(out=kmf[b0_:b1_, :],
                                                          in_=k_[b0_:b1_, :].rearrange("p (b l) -> p b l", l=256),
                                                          axis=AX.X, op=ALU.add), reads=[k_.b], writes=[kmf.b])
                fw.dve.op(lambda: nc.vector.tensor_scalar(out=kmb[b0_:b1_, :], in0=kmf[b0_:b1_, :], scalar1=1.0 / 256,
                                                          scalar2=None, op0=ALU.mult), reads=[kmf.b], writes=[kmb.b])
                for qt in range(NT):
                    fw.pe.op(lambda qt=qt: nc.tensor.matmul(pG[:, qt, :], lhsT=q_[b0_:b1_, qt * 128:(qt + 1) * 128],
                                                            rhs=kmb[b0_:b1_, :], start=True, stop=True,
                                                            skip_group_check=True),
                             reads=[q_.b, kmb.b], writes=[pG.b])
                fw.dve.op(lambda: nc.vector.tensor_tensor(out=gsb[:], in0=pG[:], in1=vb[:], op=ALU.add),
                          reads=[pG.b, vb.b], writes=[gsb.b])
                for qt in range(NT):
                    fw.dve.op(lambda qt=qt: nc.vector.max(out=m8[:, qt, :], in_=gsb[:, qt, :]),
                              reads=[gsb.b], writes=[m8.b])
                fw.dve.op(lambda: nc.vector.tensor_tensor(out=selb[:], in0=gsb[:],
                                                          in1=m8[:, :, 2:3].to_broadcast([128, NT, 16]), op=ALU.is_ge),
                          reads=[gsb.b, m8.b], writes=[selb.b])
                fw.dve.op(lambda: nc.vector.tensor_scalar(out=bb[:], in0=selb[:], scalar1=-NEG, scalar2=NEG,
                                                          op0=ALU.mult, op1=ALU.add), reads=[selb.b], writes=[bb.b])
            for qg in range(8):
                o_ = ob[(j * 8 + qg) % 2]
                for hh in range(2):
                    b0_, b1_ = hh * 64, hh * 64 + 64
                    bb = biasb[hh]
                    sT = sbT[cnt % 2]
                    cnt += 1
                    for t in range(4):
                        fw.pe.op(lambda t=t: nc.tensor.transpose(out=pT[0:16, t, :], in_=bb[:, 4 * qg + t, :],
                                                                 identity=self.ident[:]),
                                 reads=[bb.b, self.ident.b], writes=[pT.b])
                    fw.dve.op(lambda: nc.vector.tensor_copy(out=sT[:], in_=pT[0:16, :, :].rearrange("p a b -> p (a b)")),
                              reads=[pT.b], writes=[sT.b])
                    for kt in range(4 * qg + 4):
                        v = kt - 4 * qg
                        blk = kt // 2
                        bias = None
                        masks = ()
                        c0 = 0
                        if v < 0:
                            bias = (e16[:, blk, :], sT, 0, 512, [sT.b, e16.b])
                        else:
                            c0 = 128 * v
                            masks = [(c0, 128, tri, 0)]
                            if v < 2:
                                bias = (e16[:, blk, :], sT, 256, 512, [sT.b, e16.b])
                        self.att_step(st, k_[b0_:b1_, kt * 128:(kt + 1) * 128], q_[b0_:b1_, qg * 512:(qg + 1) * 512],
                                      c0, 512, [(pO, vv[:, kt, j * 128:(j + 1) * 128], [vv.b]),
                                                (pS, self.ones_bf[:], [self.ones_bf.b])],
                                      start=(kt == 0), rd=[q_.b, k_.b], bias=bias, masks=masks)
                    self.att_flush(st)
                    fw.dve.op(lambda: nc.vector.tensor_copy(out=rr[b0_:b1_, :], in_=pS[b0_:b1_, :]),
                              reads=[pS.b], writes=[rr.b])
                    fw.act.op(lambda: nc.scalar.copy(out=xo[b0_:b1_, :], in_=pO[b0_:b1_, :]), reads=[pO.b], writes=[xo.b])
                    fw.dve.op(lambda: nc.vector.reciprocal(out=rr[b0_:b1_, :], in_=rr[b0_:b1_, :]),
                              reads=[rr.b], writes=[rr.b])
                    fw.dve.op(lambda: nc.vector.tensor_tensor(out=o_[b0_:b1_, :], in0=xo[b0_:b1_, :], in1=rr[b0_:b1_, :],
                                                              op=ALU.mult), reads=[xo.b, rr.b], writes=[o_.b])
                fw.sp.dma(oT_dram[j * 128:(j + 1) * 128, qg * 512:(qg + 1) * 512], o_[:], reads=[o_.b])
            fw.barrier()
        fw.pop()


    def nsa_attn(self, FM, TM, oT_dram, pe_ap, w1_ap, w2_ap, C):
        nc, fw = self.nc, self.fw
        io = self.io
        fw.push()
        tri = fw.sb([128, 128], BF16)
        fw.sp.dma(tri[:], C["tri"], writes=[tri.b])
        wm = fw.sb([128, 128], BF16)
        fw.sp.dma(wm[:], C["nsa_wm"], writes=[wm.b])
        ttab = fw.sb([128, 3072], BF16)
        fw.sp.dma(ttab[:], C["nsa_ttab"], writes=[ttab.b])
        e64 = fw.sb([128, NT, 128], BF16)
        fw.sp.dma(e64[0:64, :, :], C["nsa_e64"], writes=[e64.b])
        fw.sp.dma(e64[64:128, :, :], C["nsa_e64"], writes=[e64.b])
        selg = fw.sb([48, 48, 128], BF16)
        fw.sp.dma(selg[:], C["nsa_selg"], writes=[selg.b])
        ta = fw.sb([128, NT, 64], F32)
        tb = fw.sb([128, NT, 64], F32)
        fw.sp.dma(ta[:], C["nsa_ta"].rearrange("p (t b) -> p t b", b=64), writes=[ta.b])
        fw.sp.dma(tb[:], C["nsa_tb"].rearrange("p (t b) -> p t b", b=64), writes=[tb.b])
        id64 = fw.sb([128, 64], BF16)
        fw.sp.dma(id64[:], C["nsa_id64"], writes=[id64.b])
        gT = fw.sb([48, S], BF16)
        fw.sp.dma(gT[:], FM[2048:2096, :], writes=[gT.b])
        kcmpT = [fw.sb([128, 256], BF16) for _ in range(4)]
        vaug = [[fw.sb([128, 2, 128], BF16) for _ in range(2)] for _ in range(4)]
        ovf = fw.sb([128, 2, 64], BF16)
        fw.sp.dma(ovf[:], C["nsa_ov"].rearrange("(a p) b -> p a b", p=128), writes=[ovf.b])
        for g in range(4):
            fw.dve.op(lambda g=g: nc.vector.memset(kcmpT[g][:], 0.0), writes=[kcmpT[g].b])
            for vr in range(2):
                va = vaug[g][vr]
                fw.dve.op(lambda va=va: nc.vector.memset(va[:], 0.0), writes=[va.b])
                oc = 64 if vr == 0 else 0
                fw.dve.op(lambda va=va, oc=oc: nc.vector.tensor_copy(out=va[:, :, oc:oc + 64], in_=ovf[:]),
                          reads=[ovf.b], writes=[va.b])
        import os
        fw.push()
        _CS = os.environ.get("NSA_CSKIP", "")
        xT = fw.sb([128, 2, 2, S + 16], BF16)
        fw.dve.op(lambda: nc.vector.memset(xT[:, :, :, S:S + 16], 0.0), writes=[xT.b])
        fw.sp.dma(xT[:, 0, :, 0:S], FM[1024:1280, :].rearrange("(c p) t -> p c t", p=128), writes=[xT.b])
        fw.sp.dma(xT[:, 1, :, 0:S], FM[1280:1536, :].rearrange("(c p) t -> p c t", p=128), writes=[xT.b])
        w1sb = [fw.sb([128, 32, 256], BF16) for _ in range(2)]
        w2f = [fw.sb([128, 2, 64], BF16) for _ in range(2)]
        w2d = [fw.sb([128, 2, 128], BF16) for _ in range(2)]
        pef = fw.sb([128, 2, 32], F32)
        peb = fw.sb([128, 2, 32], BF16)
        for i in range(2):
            for hb in range(2):
                fw.pool.dma(w1sb[i][hb * 64:(hb + 1) * 64, :, :], w1_ap[i].rearrange("(t d) n -> d t n", d=64),
                            writes=[w1sb[i].b])
                fw.sp.dma(pef[hb * 64:(hb + 1) * 64, i, :], pe_ap[i].rearrange("t d -> d t"), writes=[pef.b],
                          allow_slow_non_contiguous=True)
            fw.pool.dma(w2f[i][:], w2_ap[i].rearrange("(a p) d -> p a d", p=128), writes=[w2f[i].b])
            for hb in range(2):
                fw.dve.op(lambda i=i, hb=hb: nc.vector.tensor_copy(out=w2d[i][:, :, hb * 64:(hb + 1) * 64], in_=w2f[i][:]),
                          reads=[w2f[i].b], writes=[w2d[i].b])
        fw.dve.op(lambda: nc.vector.tensor_copy(out=peb[:], in_=pef[:]), reads=[pef.b], writes=[peb.b])
        pb = fw.ps([128, 4], F32)
        b1 = fw.sb([128, 4], F32)
        for i in (range(2) if "b1" not in _CS else ()):
            for a in range(2):
                for t in range(32):
                    fw.pe.op(lambda i=i, a=a, t=t: nc.tensor.matmul(pb[:, 2 * i + a:2 * i + a + 1],
                                                                    lhsT=w1sb[i][0:64, t, a * 128:(a + 1) * 128],
                                                                    rhs=peb[0:64, i, t:t + 1], start=(t == 0 and i == 0 and a == 0),
                                                                    stop=False, skip_group_check=True),
                             reads=[w1sb[i].b, peb.b], writes=[pb.b])
        fw.dve.op(lambda: nc.vector.tensor_copy(out=b1[:], in_=pb[:]), reads=[pb.b], writes=[b1.b])
        ph = [fw.ps([128, 256], F32) for _ in range(2)]
        pk = fw.ps([128, 256], F32)
        pv = fw.ps([128, 2, 64], F32)
        hsb = [fw.sb([128, 2, 256], BF16) for _ in range(2)]
        cc_ = 0
        for i in (range(2) if "mlp" not in _CS else ()):
            for g in range(4):
                c, base = g // 2, (g % 2) * 64
                xv = xT[base:base + 64, i, c, :].rearrange("p (n s) -> p n s", s=16)
                hs = hsb[(i * 4 + g) % 2]
                fw.dve.op(lambda hs=hs: nc.vector.memset(hs[:], 0.0), writes=[hs.b])
                for a in range(2):
                    p_ = ph[cc_ % 2]
                    cc_ += 1
                    for t in range(32):
                        rhs = xv[:, 0:256, t] if t < 16 else xv[:, 1:257, t - 16]
                        fw.pe.op(lambda t=t, rhs=rhs, a=a: nc.tensor.matmul(p_[:, 0:256],
                                                                            lhsT=w1sb[i][base:base + 64, t, a * 128:(a + 1) * 128],
                                                                            rhs=rhs, start=(t == 0), stop=(t == 31)),
                                 reads=[xT.b, w1sb[i].b], writes=[p_.b])
                    fw.act.op(lambda a=a, p_=p_: nc.scalar.activation(out=hs[:, a, 0:256], in_=p_[:, 0:256], func=AF.Silu,
                                                                      bias=b1[:, 2 * i + a:2 * i + a + 1]),
                              reads=[p_.b, b1.b], writes=[hs.b])
                if i == 0:
                    for a in range(2):
                        fw.pe.op(lambda a=a: nc.tensor.matmul(pk[:, 0:256], lhsT=w2d[0][:, a, :], rhs=hs[:, a, 0:256],
                                                              start=(a == 0), stop=(a == 1)),
                                 reads=[w2d[0].b, hs.b], writes=[pk.b])
                    fw.dve.op(lambda g=g: nc.vector.tensor_copy(out=kcmpT[g][:, 0:256], in_=pk[:, 0:256]),
                              reads=[pk.b], writes=[kcmpT[g].b])
                else:
                    for ntile in range(2):
                        for a in range(2):
                            fw.pe.op(lambda a=a, ntile=ntile: nc.tensor.matmul(pv[:, ntile, :],
                                                                               lhsT=hs[:, a, ntile * 128:(ntile + 1) * 128],
                                                                               rhs=w2f[1][:, a, :], start=(a == 0), stop=(a == 1),
                                                                               skip_group_check=True),
                                     reads=[w2f[1].b, hs.b], writes=[pv.b])
                    for vr in range(2):
                        va = vaug[g][vr]
                        vc0 = 0 if vr == 0 else 64
                        fw.dve.op(lambda va=va, vc0=vc0: nc.vector.tensor_copy(out=va[:, :, vc0:vc0 + 64], in_=pv[:]),
                                  reads=[pv.b], writes=[va.b])
        fw.pop()
        st = self.att_state()
        pO = fw.ps([128, 512], F32)
        pS = fw.ps([128, 512], F32)
        pGt = fw.ps([128, 512], F32)
        pI = fw.ps([128, 512], F32)
        pTb = fw.ps([128, 4, 128], BF16)
        qc = fw.sb([128, 2, S], BF16)
        ksd = fw.sb([128, S], BF16)
        kwd = fw.sb([128, S], BF16)
        vsd = fw.sb([128, NT, 128], BF16)
        vwd = fw.sb([128, NT, 128], BF16)
        stg = [fw.sb([128, 8, 512], BF16) for _ in range(2)]
        acc = [fw.sb([128, 512], F32) for _ in range(4)]
        impE = fw.sb([128, 512], F32)
        impH = fw.sb([128, 512], BF16)
        impL = fw.sb([128, 512], BF16)
        xs = fw.sb([128, 512], F32)
        rr = fw.sb([128, 512], F32)
        t1 = fw.sb([128, 64], F32)
        t2 = fw.sb([128, 64], F32)
        m8a = fw.sb([128, 8], F32)
        m8b = fw.sb([128, 8], F32)
        selb = fw.sb([128, 64], F32)
        bb = fw.sb([128, 128], BF16)
        sbT = fw.sb([128, 512], BF16)
        ob = [fw.sb([128, 512], BF16) for _ in range(4)]
        tm_view = TM.rearrange("(t p) c -> p t c", p=128)
        nstg = 0

        def gate_mul(r, h, br, base, qg, first):
            fw.pe.op(lambda: nc.tensor.matmul(pGt[:], lhsT=selg[:, h * 3 + br, :], rhs=gT[:, qg * 512:(qg + 1) * 512],
                                              start=True, stop=True), reads=[selg.b, gT.b], writes=[pGt.b])
            if first:
                fw.dve.op(lambda: nc.vector.tensor_tensor(out=acc[r][base:base + 64, :], in0=xs[base:base + 64, :],
                                                          in1=pGt[base:base + 64, :], op=ALU.mult),
                          reads=[xs.b, pGt.b], writes=[acc[r].b])
            else:
                fw.dve.op(lambda: nc.vector.tensor_tensor(out=xs[base:base + 64, :], in0=xs[base:base + 64, :],
                                                          in1=pGt[base:base + 64, :], op=ALU.mult),
                          reads=[xs.b, pGt.b], writes=[xs.b])
                fw.pool.op(lambda: nc.gpsimd.tensor_tensor(out=acc[r][base:base + 64, :], in0=acc[r][base:base + 64, :],
                                                           in1=xs[base:base + 64, :], op=ALU.add),
                           reads=[xs.b, acc[r].b], writes=[acc[r].b])

        def normalize(lo, hi, guard):
            if guard:
                fw.dve.op(lambda: nc.vector.tensor_scalar(out=rr[lo:hi, :], in0=pS[lo:hi, :], scalar1=1e-30, scalar2=None,
                                                          op0=ALU.max), reads=[pS.b], writes=[rr.b])
            else:
                fw.dve.op(lambda: nc.vector.tensor_copy(out=rr[lo:hi, :], in_=pS[lo:hi, :]), reads=[pS.b], writes=[rr.b])
            fw.act.op(lambda: nc.scalar.copy(out=xs[lo:hi, :], in_=pO[lo:hi, :]), reads=[pO.b], writes=[xs.b])
            fw.dve.op(lambda: nc.vector.reciprocal(out=rr[lo:hi, :], in_=rr[lo:hi, :]), reads=[rr.b], writes=[rr.b])
            fw.dve.op(lambda: nc.vector.tensor_tensor(out=xs[lo:hi, :], in0=xs[lo:hi, :], in1=rr[lo:hi, :], op=ALU.mult),
                      reads=[xs.b, rr.b], writes=[xs.b])

        import os
        _G = [int(v) for v in os.environ.get("NSA_G", "0,1,2,3").split(",")]
        _QG = [int(v) for v in os.environ.get("NSA_QG", "0,1,2,3,4,5,6,7").split(",")]
        _SKIP = set(os.environ.get("NSA_SKIP", "").split(","))
        for g in _G:
            kc_, kb_ = 12 + g // 2, (g % 2) * 64
            fw.sp.dma(qc[:], FM[g * 256:(g + 1) * 256, :].rearrange("(c p) t -> p c t", p=128), writes=[qc.b])
            for hb in range(2):
                fw.sp.dma(ksd[hb * 64:(hb + 1) * 64, :], FM[1536 + g * 64:1536 + (g + 1) * 64, :], writes=[ksd.b])
                fw.sp.dma(kwd[hb * 64:(hb + 1) * 64, :], FM[1792 + g * 64:1792 + (g + 1) * 64, :], writes=[kwd.b])
            for t0 in range(0, NT, 8):
                sg_ = stg[nstg % 2]
                nstg += 1
                fw.sp.dma(sg_[:], tm_view[:, t0:t0 + 8, 0:512], writes=[sg_.b])
                for hb in range(2):
                    fw.pool.op(lambda sg_=sg_, hb=hb, t0=t0: nc.gpsimd.tensor_copy(
                        out=vsd[:, t0:t0 + 8, hb * 64:(hb + 1) * 64], in_=sg_[:, :, g * 64:(g + 1) * 64]),
                        reads=[sg_.b], writes=[vsd.b])
                    fw.pool.op(lambda sg_=sg_, hb=hb, t0=t0: nc.gpsimd.tensor_copy(
                        out=vwd[:, t0:t0 + 8, hb * 64:(hb + 1) * 64], in_=sg_[:, :, 256 + g * 64:256 + (g + 1) * 64]),
                        reads=[sg_.b], writes=[vwd.b])
            for qg in _QG:
                for r in (range(4) if "cmp" not in _SKIP else ()):
                    h = 4 * g + r
                    base = (r % 2) * 64
                    oth = 64 - base
                    q_ = qc[base:base + 64, r // 2, qg * 512:(qg + 1) * 512]
                    first = True
                    for ntile in range(2):
                        s_ = 512 * qg - 2048 * ntile
                        if s_ < 0:
                            continue
                        masks = [(0, 512, ttab, s_)] if s_ < 2560 else ()
                        self.att_step(st, kcmpT[g][base:base + 64, ntile * 128:(ntile + 1) * 128], q_, 0, 512,
                                      [(pO, vaug[g][r % 2][:, ntile, :], [vaug[g][r % 2].b]),
                                       (pS, self.ones_bf[:], [self.ones_bf.b])],
                                      start=first, rd=[qc.b, kcmpT[g].b], masks=masks)
                        first = False
                    self.att_flush(st)
                    normalize(0, 128, True)
                    if r < 2:
                        fw.pool.op(lambda oth=oth: nc.gpsimd.tensor_copy(out=impE[oth:oth + 64, :], in_=xs[oth:oth + 64, :]),
                                   reads=[xs.b], writes=[impE.b])
                    else:
                        fw.pool.op(lambda oth=oth: nc.gpsimd.tensor_tensor(out=impE[oth:oth + 64, :], in0=impE[oth:oth + 64, :],
                                                                           in1=xs[oth:oth + 64, :], op=ALU.add),
                                   reads=[xs.b, impE.b], writes=[impE.b])
                    gate_mul(r, h, 0, base, qg, True)
                for r in (range(4) if "win" not in _SKIP else ()):
                    h = 4 * g + r
                    base = (r % 2) * 64
                    q_ = qc[base:base + 64, r // 2, qg * 512:(qg + 1) * 512]
                    first = True
                    for m in range(-4, 4):
                        kt = 4 * qg + m
                        if kt < 0:
                            continue
                        if m < 0:
                            c0, c1 = 0, 128 * (m + 5)
                            masks = [(c1 - 128, 128, wm, 0)]
                        else:
                            c0, c1 = 128 * m, 512
                            masks = [(c0, 128, tri, 0)]
                        self.att_step(st, kwd[base:base + 64, kt * 128:(kt + 1) * 128], q_, c0, c1,
                                      [(pO, vwd[:, kt, :], [vwd.b]), (pS, self.ones_bf[:], [self.ones_bf.b])],
                                      start=first, rd=[qc.b, kwd.b], masks=masks)
                        first = False
                    self.att_flush(st)
                    normalize(base, base + 64, False)
                    gate_mul(r, h, 2, base, qg, False)
                for t in (range(4) if "topk" not in _SKIP else ()):
                    qt = 4 * qg + t
                    pi = pI[:, t * 64:(t + 1) * 64]
                    if t == 0:
                        fw.dve.op(lambda: nc.vector.tensor_copy(out=impH[:], in_=impE[:]), reads=[impE.b], writes=[impH.b])
                        fw.dve.op(lambda: nc.vector.tensor_tensor(out=impL[:], in0=impE[:], in1=impH[:], op=ALU.subtract),
                                  reads=[impE.b, impH.b], writes=[impL.b])
                    for k4, src_ in enumerate((impH, impL)):
                        fw.pe.op(lambda src_=src_, k4=k4, t=t, pi=pi: nc.tensor.matmul(
                            pi, lhsT=src_[:, t * 128:(t + 1) * 128], rhs=id64[:, :],
                            start=(k4 == 0), stop=(k4 == 1), skip_group_check=True),
                            reads=[src_.b, id64.b], writes=[pI.b])
                    _LV = int(os.environ.get("TOPK_LEVEL", 9))
                    if _LV < 2:
                        continue
                    fw.dve.op(lambda pi=pi, qt=qt: nc.vector.tensor_tensor(out=t1[:], in0=pi, in1=ta[:, qt, :], op=ALU.mult),
                              reads=[pI.b, ta.b], writes=[t1.b])

                    fw.dve.op(lambda qt=qt: nc.vector.tensor_tensor(out=t1[:], in0=t1[:], in1=tb[:, qt, :], op=ALU.add),
                              reads=[t1.b, tb.b], writes=[t1.b])
                    if _LV < 3:
                        continue
                    fw.dve.op(lambda: nc.vector.max(out=m8a[:], in_=t1[:]), reads=[t1.b], writes=[m8a.b])
                    if _LV < 4:
                        continue
                    fw.dve.op(lambda: nc.vector.tensor_tensor(out=t2[:], in0=t1[:], in1=m8a[:, 7:8].to_broadcast([128, 64]),
                                                              op=ALU.is_ge), reads=[t1.b, m8a.b], writes=[t2.b])
                    fw.dve.op(lambda: nc.vector.scalar_tensor_tensor(out=t2[:], in0=t2[:], scalar=-1.0e10, in1=t1[:],
                                                                     op0=ALU.mult, op1=ALU.add),
                              reads=[t1.b, t2.b], writes=[t2.b])
                    fw.dve.op(lambda: nc.vector.max(out=m8b[:], in_=t2[:]), reads=[t2.b], writes=[m8b.b])
                    fw.dve.op(lambda: nc.vector.tensor_tensor(out=selb[:], in0=t1[:], in1=m8b[:, 7:8].to_broadcast([128, 64]),
                                                              op=ALU.is_ge), reads=[t1.b, m8b.b], writes=[selb.b])
                    fw.dve.op(lambda: nc.vector.tensor_scalar(out=bb[:, 0:64], in0=selb[:], scalar1=-NEG, scalar2=NEG,
                                                              op0=ALU.mult, op1=ALU.add), reads=[selb.b], writes=[bb.b])
                    fw.dve.op(lambda: nc.vector.tensor_scalar(out=bb[:, 64:128], in0=selb[:], scalar1=-NEG, scalar2=NEG,
                                                              op0=ALU.mult, op1=ALU.add), reads=[selb.b], writes=[bb.b])
                    if _LV < 5:
                        continue
                    fw.pe.op(lambda t=t: nc.tensor.transpose(out=pTb[:, t, :], in_=bb[:], identity=self.ident[:]),
                             reads=[bb.b, self.ident.b], writes=[pTb.b])
                fw.dve.op(lambda: nc.vector.tensor_copy(out=sbT[:], in_=pTb[:, :, :].rearrange("p a b -> p (a b)")),
                          reads=[pTb.b], writes=[sbT.b])
                for r in (range(4) if "sel" not in _SKIP else ()):
                    h = 4 * g + r
                    base = (r % 2) * 64
                    q_ = qc[base:base + 64, r // 2, qg * 512:(qg + 1) * 512]
                    for kt in range(4 * qg + 4):
                        v = kt - 4 * qg
                        c0 = 128 * v if v > 0 else 0
                        masks = [(c0, 128, tri, 0)] if v >= 0 else ()
                        self.att_step(st, ksd[base:base + 64, kt * 128:(kt + 1) * 128], q_, c0, 512,
                                      [(pO, vsd[:, kt, :], [vsd.b]), (pS, self.ones_bf[:], [self.ones_bf.b])],
                                      start=(kt == 0), rd=[qc.b, ksd.b],
                                      bias=(e64[base:base + 64, kt, :], sbT[base:base + 64, :], c0, 512, [sbT.b, e64.b]),
                                      masks=masks)
                    self.att_flush(st)
                    normalize(base, base + 64, False)
                    gate_mul(r, h, 1, base, qg, False)
                for cc2 in range(2):
                    o_ = ob[(qg * 2 + cc2) % 4]
                    for hb in range(2):
                        r = cc2 * 2 + hb
                        fw.act.op(lambda r=r, hb=hb, o_=o_: nc.scalar.copy(out=o_[hb * 64:(hb + 1) * 64, :],
                                                                          in_=acc[r][hb * 64:(hb + 1) * 64, :]),
                                  reads=[acc[r].b], writes=[o_.b])
                    fw.sp.dma(oT_dram[(2 * g + cc2) * 128:(2 * g + cc2 + 1) * 128, qg * 512:(qg + 1) * 512], o_[:],
                              reads=[o_.b])
            fw.barrier()
        fw.pop()


def host_consts():
    pos = np.arange(S, dtype=np.float32)
    half = 8
    inv_freq = (np.float32(500000.0) ** (-np.arange(half, dtype=np.float32) / half)).astype(np.float32)
    ang = (pos[:, None] * inv_freq[None, :]).astype(np.float32)
    cos = np.cos(ang).astype(np.float32)
    sin = np.sin(ang).astype(np.float32)
    c = {}
    c["rope_cc"] = np.concatenate([cos, cos], axis=1)
    c["rope_ss"] = np.concatenate([-sin, sin], axis=1)
    p = np.arange(128)
    c["tri"] = (p[:, None] <= p[None, :]).astype(np.float32).astype(ml_dtypes.bfloat16)
    vb = np.zeros((128, NT, 16), np.float32)
    for qt in range(NT):
        vb[:, qt, qt // 2:] = -1e30
    c["moba_vb"] = vb.reshape(128, NT * 16)
    e16 = np.zeros((16, 16, 128), np.float32)
    for b in range(16):
        e16[b, b, :] = 1.0
    c["moba_e16"] = e16.astype(ml_dtypes.bfloat16)
    c["nsa_wm"] = (p[None, :] < p[:, None]).astype(np.float32).astype(ml_dtypes.bfloat16)
    jj = np.arange(3072)
    c["nsa_ttab"] = (jj[None, :] >= 16 * p[:, None] + 31).astype(np.float32).astype(ml_dtypes.bfloat16)
    e64 = np.zeros((64, NT, 128), np.float32)
    for kt in range(NT):
        e64[2 * kt, kt, 0:64] = 1.0
        e64[2 * kt + 1, kt, 64:128] = 1.0
    c["nsa_e64"] = e64.astype(ml_dtypes.bfloat16)
    sg = np.zeros((48, 48, 128), np.float32)
    for rr_ in range(48):
        sg[rr_, rr_, :] = 1.0
    c["nsa_selg"] = sg.astype(ml_dtypes.bfloat16)
    ta = np.zeros((128, NT, 64), np.float32)
    tb = np.zeros((128, NT, 64), np.float32)
    blk = np.arange(64)
    for qt in range(NT):
        for pp in range(128):
            qb = (qt * 128 + pp) // 64
            valid = blk <= qb
            a_ = valid.astype(np.float32)
            b_ = np.where(valid, 0.0, -1e30).astype(np.float32)
            for jf, val in ((qb - 1, 1e9), (qb, 2e9), (0, 3e9)):
                if jf >= 0:
                    a_[jf] = 0.0
                    b_[jf] = val
            ta[pp, qt] = a_
            tb[pp, qt] = b_
    c["nsa_ta"] = ta.reshape(128, NT * 64)
    c["nsa_tb"] = tb.reshape(128, NT * 64)
    n = np.arange(256)
    cs = n[:, None] * 16
    bs = blk[None, :] * 64
    ov = ((cs < bs + 64) & (cs + 32 > bs)).astype(np.float32)
    ov[255, :] = 0.0
    c["nsa_ov"] = ov.astype(ml_dtypes.bfloat16)
    c["nsa_id64"] = ((p[:, None] % 64) == np.arange(64)[None, :]).astype(np.float32).astype(ml_dtypes.bfloat16)
    return c


INPUT_SHAPES = {
    "x": [S, D], "norm_g": [DEPTH, 6, D], "ffn_w_in": [DEPTH, 2, D, 2 * DFF], "ffn_w_out": [DEPTH, 2, DFF, D],
    "diff_w_in": [2, D, 3072], "diff_w_out": [2, D, D], "diff_lambda": [2, 4, 64], "diff_subln": [2, 128],
    "moba_w_in": [1, D, 3072], "moba_w_out": [1, D, D],
    "nsa_w_in": [1, D, 2608], "nsa_w_out": [1, D, D], "nsa_cmp_pe": [1, 2, 32, 64],
    "nsa_cmp_w1": [1, 2, 2048, 256], "nsa_cmp_w2": [1, 2, 256, 64],
}


def build_program(phases, used=None):
    nc = bass.Bass("TRN2", target_bir_lowering=False)
    io = {}
    for name, shape in INPUT_SHAPES.items():
        if used is not None and name not in used:
            continue
        io[name] = nc.dram_tensor(name, list(shape), F32, kind="ExternalInput").ap()
    for name, arr in host_consts().items():
        dt = BF16 if arr.dtype == ml_dtypes.bfloat16 else F32
        io[name] = nc.dram_tensor(name, list(arr.shape), dt, kind="ExternalInput").ap()
    io["out"] = nc.dram_tensor("out", [S, D], F32, kind="ExternalOutput").ap()
    io["H"] = nc.dram_tensor("H", [S, D], F32, kind="Internal").ap()
    io["FM"] = nc.dram_tensor("FM", [2560, S], BF16, kind="Internal").ap()
    io["TM"] = nc.dram_tensor("TM", [S, 1024], BF16, kind="Internal").ap()
    io["OT"] = nc.dram_tensor("OT", [D, S], BF16, kind="Internal").ap()
    fw = FW(nc)
    pg = Prog(nc, fw, io)
    phases(pg, io)
    fw.close()
    return nc, fw


def lambda_init_of(i):
    return 0.8 - 0.6 * math.exp(-0.3 * i)


def mixer_diff(pg, io, i, src, dst):
    j = i // 3
    segs = [("rope", k) for k in range(8)] + [("tm", k * 256) for k in range(4)]
    pg.proj(src, io["diff_w_in"][j], 3072, io["norm_g"][i, 2], segs, io["FM"][0:2048, :], io["TM"],
            (io["rope_cc"], io["rope_ss"]))
    pg.diff_attn(io["FM"][0:2048, :], io["TM"], io["OT"], io["diff_lambda"][j], io["diff_subln"][j],
                 lambda_init_of(i), io["tri"])
    pg.outproj(src, dst, io["OT"], io["diff_w_out"][j], io["norm_g"][i, 3])


def mixer_moba(pg, io, i, src, dst):
    j = i // 3
    segs = [("rope", k) for k in range(8)] + [("tm", k * 256) for k in range(4)]
    pg.proj(src, io["moba_w_in"][j], 3072, io["norm_g"][i, 2], segs, io["FM"][0:2048, :], io["TM"],
            (io["rope_cc"], io["rope_ss"]))
    pg.moba_attn(io["FM"][0:2048, :], io["TM"], io["OT"], io["tri"], io["moba_vb"], io["moba_e16"])
    pg.outproj(src, dst, io["OT"], io["moba_w_out"][j], io["norm_g"][i, 3])


def mixer_nsa(pg, io, i, src, dst):
    j = i // 3
    segs = [("rope", 0), ("rope", 1), ("rope", 2), ("rope", 3), ("rope", 4), ("fm", 5), ("rope", 6), ("tm", 0),
            ("rope", 7), ("tm", 256), ("gate", 8)]
    pg.proj(src, io["nsa_w_in"][j], 2608, io["norm_g"][i, 2], segs, io["FM"][0:2304, :], io["TM"],
            (io["rope_cc"], io["rope_ss"]))
    pg.nsa_attn(io["FM"], io["TM"], io["OT"], io["nsa_cmp_pe"][j], io["nsa_cmp_w1"][j], io["nsa_cmp_w2"][j], io)
    pg.outproj(src, dst, io["OT"], io["nsa_w_out"][j], io["norm_g"][i, 3])


MIXERS = {0: mixer_diff, 1: mixer_moba, 2: mixer_nsa}


def full_phases(pg, io):
    H = io["H"]
    for i in range(DEPTH):
        src = io["x"] if i == 0 else H
        pg.ffn(src, H, io["ffn_w_in"][i, 0], io["ffn_w_out"][i, 0], io["norm_g"][i, 0], io["norm_g"][i, 1])
        MIXERS[i % 3](pg, io, i, H, H)
        dst = io["out"] if i == DEPTH - 1 else H
        pg.ffn(H, dst, io["ffn_w_in"][i, 1], io["ffn_w_out"][i, 1], io["norm_g"][i, 4], io["norm_g"][i, 5])


_CACHE = {}


def kernel(**inputs):
    if "nc" not in _CACHE:
        _CACHE["nc"] = build_program(full_phases)[0]
        _CACHE["consts"] = host_consts()
    nc = _CACHE["nc"]
    consts = _CACHE["consts"]
    arrs = {k: np.ascontiguousarray(np.asarray(v), dtype=np.float32) for k, v in inputs.items()}
    n = arrs["x"].shape[0]
    in_maps = []
    for b in range(n):
        m = {k: v for k, v in arrs.items() if k != "x"}
        m["x"] = arrs["x"][b]
        m.update(consts)
        in_maps.append(m)
    res = run_bass_kernel_spmd(nc, in_maps, core_ids=list(range(n)))
    return np.stack([np.asarray(r["out"], dtype=np.float32) for r in res.results], axis=0)
```
